# Optimizing a Trainium2 kernel written in Bass

```python
import jax, jax.numpy as jnp
from jax import lax
import numpy as np

D_MODEL = 1024
BATCH = 16
SEQ = 2048
DEPTH = 2

HEAD_DIM = 64
N_GROUPS = 4
GROUP_WIDTH = D_MODEL // N_GROUPS
GROUP_HEADS = GROUP_WIDTH // HEAD_DIM
MIX_WIDTH = N_GROUPS * GROUP_WIDTH
IN_COLS = 9 * GROUP_WIDTH
MOBA_BLOCK = 256
MOBA_TOPK = 3
MOBA_QCHUNK = 16
DILATED_CFGS = ((128, 1), (512, 4), (2048, 16))
BAND = 128
CONV_WIDTH = 31
FFN_CONV_WIDTH = 3
D_FF = 2816
N_MEM = 256
ROPE_THETA = 10000.0
EPS = 1e-6
ATTN_SCALE = HEAD_DIM ** -0.5

kernel_name = 'hybrid_moba_dilated_conformer_block'


def rms_norm(x, g):
    xf = x.astype(jnp.float32)
    y = xf * lax.rsqrt(jnp.mean(xf * xf, axis=-1, keepdims=True) + EPS)
    return (y * g.astype(jnp.float32)).astype(x.dtype)


def layer_norm(x, g, b):
    xf = x.astype(jnp.float32)
    mu = jnp.mean(xf, axis=-1, keepdims=True)
    var = jnp.mean(jnp.square(xf - mu), axis=-1, keepdims=True)
    y = (xf - mu) * lax.rsqrt(var + EPS)
    return (y * g.astype(jnp.float32) + b.astype(jnp.float32)).astype(x.dtype)


def rope_tables(seq):
    inv = ROPE_THETA ** (-jnp.arange(0, HEAD_DIM, 2, dtype=jnp.float32) / HEAD_DIM)
    ang = jnp.arange(seq, dtype=jnp.float32)[:, None] * inv[None, :]
    ang = jnp.concatenate([ang, ang], axis=-1)
    return jnp.cos(ang), jnp.sin(ang)


def apply_rope(x, cos, sin):
    half = HEAD_DIM // 2
    xf = x.astype(jnp.float32)
    rot = jnp.concatenate([-xf[..., half:], xf[..., :half]], axis=-1)
    return (xf * cos + rot * sin).astype(x.dtype)


def split_heads(t, n_heads):
    b, s, _ = t.shape
    return t.reshape(b, s, n_heads, HEAD_DIM).transpose(0, 2, 1, 3)


def merge_heads(t):
    b, h, s, d = t.shape
    return t.transpose(0, 2, 1, 3).reshape(b, s, h * d)


def causal_depthwise_conv(x, w, b):
    k, c = w.shape
    y = lax.conv_general_dilated(x, w[:, None, :].astype(x.dtype), window_strides=(1,),
                                 padding=[(k - 1, 0)], dimension_numbers=('NWC', 'WIO', 'NWC'),
                                 feature_group_count=c)
    return y + b.astype(x.dtype)


def moba_attention(q, k, v):
    b, h, s, d = q.shape
    nb = -(-s // MOBA_BLOCK)
    sp = nb * MOBA_BLOCK
    pad = ((0, 0), (0, 0), (0, sp - s), (0, 0))
    kb = jnp.pad(k, pad).reshape(b, h, nb, MOBA_BLOCK, d)
    vb = jnp.pad(v, pad).reshape(b, h, nb, MOBA_BLOCK, d)
    kmean = jnp.mean(kb.astype(jnp.float32), axis=3)
    qpos = jnp.arange(s)
    qblk = qpos // MOBA_BLOCK
    gate = jnp.einsum('bhsd,bhnd->bhsn', q.astype(jnp.float32), kmean)
    fully_past = jnp.arange(nb)[None, :] < qblk[:, None]
    gate = jnp.where(fully_past, gate, -jnp.inf)
    kk = min(MOBA_TOPK, nb)
    _, top_idx = lax.top_k(gate, kk)
    own = jnp.broadcast_to(qblk[None, None, :, None], (b, h, s, 1)).astype(top_idx.dtype)
    sel_idx = jnp.concatenate([top_idx, own], axis=-1)
    sel_valid = jnp.concatenate([jnp.arange(kk)[None, :] < jnp.minimum(MOBA_TOPK, qblk)[:, None],
                                 jnp.ones((s, 1), dtype=bool)], axis=-1)
    n_sel = kk + 1
    nc = s // MOBA_QCHUNK
    q_c = q.reshape(b, h, nc, MOBA_QCHUNK, d).transpose(2, 0, 1, 3, 4)
    idx_c = sel_idx.reshape(b, h, nc, MOBA_QCHUNK, n_sel).transpose(2, 0, 1, 3, 4)
    valid_c = sel_valid.reshape(nc, MOBA_QCHUNK, n_sel)
    pos_c = qpos.reshape(nc, MOBA_QCHUNK)
    bi = jnp.arange(b)[:, None, None, None]
    hi = jnp.arange(h)[None, :, None, None]
    offs = jnp.arange(MOBA_BLOCK)

    def chunk(args):
        qc, idx, valid, pos = args
        ks = kb[bi, hi, idx]
        vs = vb[bi, hi, idx]
        kpos = idx[..., None] * MOBA_BLOCK + offs
        mask = valid[None, None, :, :, None] & (kpos <= pos[None, None, :, None, None])
        sc = jnp.einsum('bhqd,bhqnkd->bhqnk', qc, ks).astype(jnp.float32) * ATTN_SCALE
        sc = jnp.where(mask, sc, -jnp.inf)
        p = jax.nn.softmax(sc.reshape(b, h, MOBA_QCHUNK, n_sel * MOBA_BLOCK), axis=-1)
        p = p.reshape(b, h, MOBA_QCHUNK, n_sel, MOBA_BLOCK)
        return jnp.einsum('bhqnk,bhqnkd->bhqd', p, vs.astype(jnp.float32))

    o = lax.map(chunk, (q_c, idx_c, valid_c, pos_c))
    return o.transpose(1, 2, 0, 3, 4).reshape(b, h, s, d).astype(q.dtype)


def band_attention_stats(q, k, v, window):
    b, h, g, L, d = q.shape
    nq = -(-L // BAND)
    lp = nq * BAND
    pad = ((0, 0),) * 3 + ((0, lp - L), (0, 0))
    qb = jnp.pad(q, pad).reshape(b, h, g, nq, BAND, d)
    kb = jnp.pad(k, pad).reshape(b, h, g, nq, BAND, d)
    vb = jnp.pad(v, pad).reshape(b, h, g, nq, BAND, d)
    shift = ((0, 0),) * 3 + ((1, 0), (0, 0), (0, 0))
    kband = jnp.concatenate([jnp.pad(kb, shift)[:, :, :, :nq], kb], axis=-2)
    vband = jnp.concatenate([jnp.pad(vb, shift)[:, :, :, :nq], vb], axis=-2)
    qi = jnp.arange(BAND)[:, None]
    ki = jnp.arange(2 * BAND)[None, :]
    dist = qi + BAND - ki
    first = (jnp.arange(nq) == 0)[:, None, None]
    mask = (dist >= 0) & (dist <= window) & ~(first & (ki < BAND))
    sc = jnp.einsum('bhgnqd,bhgnkd->bhgnqk', qb, kband).astype(jnp.float32) * ATTN_SCALE
    sc = jnp.where(mask, sc, -jnp.inf)
    m = jnp.max(sc, axis=-1, keepdims=True)
    p = jnp.exp(sc - m)
    l = jnp.sum(p, axis=-1)
    o = jnp.einsum('bhgnqk,bhgnkd->bhgnqd', p, vband.astype(jnp.float32))
    return (o.reshape(b, h, g, lp, d)[:, :, :, :L],
            m[..., 0].reshape(b, h, g, lp)[..., :L],
            l.reshape(b, h, g, lp)[..., :L])


def dilated_attention(q, k, v):
    b, h, s, d = q.shape
    outs, maxes, dens = [], [], []
    for window, dil in DILATED_CFGS:
        L = s // dil

        def to_res(t, L=L, dil=dil):
            return t.reshape(b, h, L, dil, d).transpose(0, 1, 3, 2, 4)

        o, m, l = band_attention_stats(to_res(q), to_res(k), to_res(v), window // dil)
        outs.append(o.transpose(0, 1, 3, 2, 4).reshape(b, h, s, d))
        maxes.append(m.transpose(0, 1, 3, 2).reshape(b, h, s))
        dens.append(l.transpose(0, 1, 3, 2).reshape(b, h, s))
    mx = jnp.stack(maxes)
    wts = jnp.exp(mx - jnp.max(mx, axis=0, keepdims=True))
    num = jnp.sum(wts[..., None] * jnp.stack(outs), axis=0)
    den = jnp.sum(wts * jnp.stack(dens), axis=0)
    return (num / den[..., None]).astype(q.dtype)


def setup_inputs(seed: int = 0) -> dict:
    key = jax.random.key(seed)
    ks = jax.random.split(key, 24)
    f32 = jnp.float32

    def nrm(k, shape, scale):
        return jax.random.normal(k, shape, f32) * scale

    def gain(k, shape):
        return 1.0 + 0.02 * jax.random.normal(k, shape, f32)

    return {
        'x': nrm(ks[0], (BATCH, SEQ, D_MODEL), 1.0),
        'mem': nrm(ks[1], (BATCH, N_MEM, D_MODEL), 1.0),
        'norm_mix': gain(ks[2], (DEPTH, D_MODEL)),
        'w_in': nrm(ks[3], (DEPTH, D_MODEL, IN_COLS), D_MODEL ** -0.5),
        'q_norm_a': gain(ks[4], (DEPTH, HEAD_DIM)),
        'k_norm_a': gain(ks[5], (DEPTH, HEAD_DIM)),
        'q_norm_b': gain(ks[6], (DEPTH, HEAD_DIM)),
        'k_norm_b': gain(ks[7], (DEPTH, HEAD_DIM)),
        'q_norm_m': gain(ks[8], (DEPTH, HEAD_DIM)),
        'k_norm_m': gain(ks[9], (DEPTH, HEAD_DIM)),
        'mem_norm': gain(ks[10], (DEPTH, D_MODEL)),
        'w_mem_kv': nrm(ks[11], (DEPTH, D_MODEL, 2 * GROUP_WIDTH), D_MODEL ** -0.5),
        'conv_w': nrm(ks[12], (DEPTH, CONV_WIDTH, GROUP_WIDTH), CONV_WIDTH ** -0.5),
        'conv_b': nrm(ks[13], (DEPTH, GROUP_WIDTH), 0.02),
        'conv_ln_g': gain(ks[14], (DEPTH, GROUP_WIDTH)),
        'conv_ln_b': nrm(ks[15], (DEPTH, GROUP_WIDTH), 0.02),
        'w_conv_out': nrm(ks[16], (DEPTH, GROUP_WIDTH, GROUP_WIDTH), GROUP_WIDTH ** -0.5),
        'out_norm': gain(ks[17], (DEPTH, MIX_WIDTH)),
        'w_out': nrm(ks[18], (DEPTH, MIX_WIDTH, D_MODEL), MIX_WIDTH ** -0.5),
        'norm_ffn': gain(ks[19], (DEPTH, D_MODEL)),
        'w_up': nrm(ks[20], (DEPTH, D_MODEL, 2 * D_FF), D_MODEL ** -0.5),
        'ffn_conv_w': nrm(ks[21], (DEPTH, FFN_CONV_WIDTH, 2 * D_FF), FFN_CONV_WIDTH ** -0.5),
        'ffn_conv_b': nrm(ks[22], (DEPTH, 2 * D_FF), 0.02),
        'w_down': nrm(ks[23], (DEPTH, D_FF, D_MODEL), D_FF ** -0.5),
    }


def reference(x, mem, norm_mix, w_in, q_norm_a, k_norm_a, q_norm_b, k_norm_b, q_norm_m, k_norm_m,
              mem_norm, w_mem_kv, conv_w, conv_b, conv_ln_g, conv_ln_b, w_conv_out, out_norm, w_out,
              norm_ffn, w_up, ffn_conv_w, ffn_conv_b, w_down):
    b, s, _ = x.shape
    cos, sin = rope_tables(s)
    splits = [GROUP_WIDTH * j for j in range(1, 9)]
    for i in range(DEPTH):
        h = rms_norm(x, norm_mix[i])
        proj = jnp.einsum('bsd,de->bse', h, w_in[i])
        qa, ka, va, qb, kb, vb, c_val, c_gate, qm = jnp.split(proj, splits, axis=-1)

        qa = apply_rope(rms_norm(split_heads(qa, GROUP_HEADS), q_norm_a[i]), cos, sin)
        ka = apply_rope(rms_norm(split_heads(ka, GROUP_HEADS), k_norm_a[i]), cos, sin)
        o_a = merge_heads(moba_attention(qa, ka, split_heads(va, GROUP_HEADS)))

        qb = apply_rope(rms_norm(split_heads(qb, GROUP_HEADS), q_norm_b[i]), cos, sin)
        kb = apply_rope(rms_norm(split_heads(kb, GROUP_HEADS), k_norm_b[i]), cos, sin)
        o_b = merge_heads(dilated_attention(qb, kb, split_heads(vb, GROUP_HEADS)))

        u = c_val * jax.nn.sigmoid(c_gate)
        u = causal_depthwise_conv(u, conv_w[i], conv_b[i])
        u = jax.nn.silu(layer_norm(u, conv_ln_g[i], conv_ln_b[i]))
        o_c = jnp.einsum('bsc,ce->bse', u, w_conv_out[i])

        mh = rms_norm(mem, mem_norm[i])
        km, vm = jnp.split(jnp.einsum('bmd,de->bme', mh, w_mem_kv[i]), 2, axis=-1)
        qm = rms_norm(split_heads(qm, GROUP_HEADS), q_norm_m[i])
        km = rms_norm(split_heads(km, GROUP_HEADS), k_norm_m[i])
        sc = jnp.einsum('bhsd,bhmd->bhsm', qm, km).astype(jnp.float32) * ATTN_SCALE
        o_m = jnp.einsum('bhsm,bhmd->bhsd', jax.nn.softmax(sc, axis=-1),
                         split_heads(vm, GROUP_HEADS).astype(jnp.float32)).astype(x.dtype)
        o_m = merge_heads(o_m)

        mix = jnp.concatenate([o_a, o_b, o_c, o_m], axis=-1).reshape(b, s, N_GROUPS, GROUP_WIDTH)
        mix = rms_norm(mix, out_norm[i].reshape(N_GROUPS, GROUP_WIDTH)).reshape(b, s, MIX_WIDTH)
        x = x + jnp.einsum('bse,ed->bsd', mix, w_out[i])

        h = rms_norm(x, norm_ffn[i])
        u = causal_depthwise_conv(jnp.einsum('bsd,df->bsf', h, w_up[i]), ffn_conv_w[i], ffn_conv_b[i])
        gate, val = jnp.split(u, 2, axis=-1)
        x = x + jnp.einsum('bsf,fd->bsd', jax.nn.silu(gate) * val, w_down[i])
    return x
```

```python
import contextlib
import numpy as np
import concourse.bass as bass
import concourse.mybir as mybir
from concourse.bass_utils import run_bass_kernel_spmd

F32 = mybir.dt.float32
BF16 = mybir.dt.bfloat16
ALU = mybir.AluOpType
AF = mybir.ActivationFunctionType
AX = mybir.AxisListType

N_CORES = 8
DEPTH = 2
S = 2048
D = 1024
NT = 16
DFF = 2816
NFT = 22
EPS = 1e-6
N_DMA_SEMS = 40
NV = 268
NEG = -30000.0
import os
ATT_LA = int(os.environ.get('ATT_LA', '1'))
RSQ_OLD = int(os.environ.get('RSQ_OLD', '1'))
QK_LN = int(os.environ.get('QK_LN', '0'))
QK_COMPACT = int(os.environ.get('QK_COMPACT', '1'))


class Reg:
    __slots__ = ("name", "lw", "readers")

    def __init__(self, name):
        self.name = name
        self.lw = None
        self.readers = []


class Op:
    __slots__ = ("eng", "fn", "deps", "dma", "needs_inc", "count", "sem", "idx")

    def __init__(self, eng, fn, dma, idx):
        self.eng = eng
        self.fn = fn
        self.dma = dma
        self.deps = {}
        self.needs_inc = False
        self.count = None
        self.sem = None
        self.idx = idx


class Prog:
    ENGS = ("pe", "act", "dve", "pool", "sp")

    def __init__(self):
        self.ops = []
        self.n_dma = 0
        self.last_on_dsem = {}
        self.last_eng = {}
        self.bar = {}

    def barrier(self):
        last = dict(self.last_eng)
        self.bar = {e: [o for f, o in last.items() if f != e] for e in self.ENGS}

    def add(self, eng, fn, reads=(), writes=(), dma=False):
        op = Op(eng, fn, dma, len(self.ops))
        for r in reads:
            if r.lw is not None:
                op.deps[r.lw] = "RAW"
        for r in writes:
            if r.lw is not None and r.lw not in op.deps:
                op.deps[r.lw] = "WAW"
            for rd in r.readers:
                if rd not in op.deps:
                    op.deps[rd] = "WAR"
        op.deps.pop(op, None)
        if self.bar.get(eng):
            for o in self.bar[eng]:
                if o.dma:
                    continue
                op.deps[o] = "BAR"
            self.bar[eng] = None
        for r in reads:
            r.readers.append(op)
        for r in writes:
            r.lw = op
            r.readers = []
        if dma:
            j = self.n_dma % N_DMA_SEMS
            self.n_dma += 1
            op.sem = j
            prev = self.last_on_dsem.get(j)
            if prev is not None and prev not in op.deps:
                op.deps[prev] = "SEM"
            self.last_on_dsem[j] = op
        else:
            self.last_eng[eng] = op
        self.ops.append(op)
        return op

    def pe(self, fn, reads=(), writes=()):
        return self.add("pe", fn, reads, writes)

    def act(self, fn, reads=(), writes=()):
        return self.add("act", fn, reads, writes)

    def dve(self, fn, reads=(), writes=()):
        return self.add("dve", fn, reads, writes)

    def pool(self, fn, reads=(), writes=()):
        return self.add("pool", fn, reads, writes)

    def dma(self, q, fn, reads=(), writes=()):
        return self.add(q, fn, reads, writes, dma=True)

    def emit(self, nc, final_wait_ops=()):
        ops = self.ops
        need = {}
        for op in ops:
            lst = []
            best = {}
            for d, kind in op.deps.items():
                if d.dma:
                    lst.append(d)
                elif d.eng == op.eng and not op.dma:
                    if op.eng == "pe":
                        continue
                    if kind == "RAW" and (d.eng not in best or best[d.eng].idx < d.idx):
                        best[d.eng] = d
                elif d.eng not in best or best[d.eng].idx < d.idx:
                    best[d.eng] = d
            for d in best.values():
                d.needs_inc = True
                lst.append(d)
            need[op] = lst
        cnt = {e: 0 for e in self.ENGS}
        dcnt = {}
        for op in ops:
            if op.dma:
                dcnt[op.sem] = dcnt.get(op.sem, 0) + 16
                op.count = dcnt[op.sem]
            elif op.needs_inc:
                cnt[op.eng] += 1
                op.count = cnt[op.eng]
        per_eng = {e: [o for o in ops if o.eng == e] for e in self.ENGS}
        with contextlib.ExitStack() as st:
            esem = {e: st.enter_context(nc.semaphore("s_" + e)) for e in self.ENGS}
            dsem = [st.enter_context(nc.semaphore("d%d" % i)) for i in range(N_DMA_SEMS)]
            block = st.enter_context(nc.Block())

            def run(ename, eng):
                seen = {}
                for op in per_eng[ename]:
                    for d in need[op]:
                        if d.dma:
                            key = ("d", d.sem)
                            sem = dsem[d.sem]
                        else:
                            key = ("e", d.eng)
                            sem = esem[d.eng]
                        if seen.get(key, 0) >= d.count:
                            continue
                        seen[key] = d.count
                        eng.wait_ge(sem, d.count)
                    ins = op.fn(eng)
                    if op.dma:
                        ins.then_inc(dsem[op.sem], 16)
                    elif op.needs_inc:
                        ins.then_inc(esem[ename], 1)
                for d in final_wait_ops:
                    if d.eng == ename:
                        key = ("d", d.sem)
                        if seen.get(key, 0) < d.count:
                            seen[key] = d.count
                            eng.wait_ge(dsem[d.sem], d.count)

            @block.tensor
            def _(eng):
                run("pe", eng)

            @block.scalar
            def _(eng):
                run("act", eng)

            @block.vector
            def _(eng):
                run("dve", eng)

            @block.gpsimd
            def _(eng):
                run("pool", eng)

            @block.sync
            def _(eng):
                run("sp", eng)


class Arena:
    def __init__(self, t, lo, hi):
        self.t = t
        self.lo = lo
        self.hi = hi
        self.off = lo

    def mark(self):
        return self.off

    def reset(self, m=None):
        self.off = self.lo if m is None else m

    def alloc(self, cols, name="a"):
        cols_al = (cols + 1) // 2 * 2
        assert self.off + cols_al <= self.hi, (name, self.off, cols_al, self.hi)
        ap = self.t[:, self.off:self.off + cols]
        self.off += cols_al
        return ap, Reg(name)

    def alloc_f32(self, cols, name="a"):
        ap, r = self.alloc(2 * cols, name)
        return ap.bitcast(F32), r


def bview(ap2d, inner):
    a = ap2d.ap
    return bass.AP(ap2d.tensor, ap2d.offset, [list(a[0]), list(a[1]), [0, inner]])


def bview_mid(ap2d, g, n):
    a = ap2d.ap
    return bass.AP(ap2d.tensor, ap2d.offset, [list(a[0]), [n, g], [0, n], [1, n]])


def bview_in(ap2d, g, n):
    a = ap2d.ap
    return bass.AP(ap2d.tensor, ap2d.offset, [list(a[0]), [n, g], [1, n], [0, n]])


def _consts():
    half = 32
    inv = (10000.0 ** (-np.arange(0, 64, 2, dtype=np.float32) / np.float32(64))).astype(np.float32)
    ang = np.arange(S, dtype=np.float32)[:, None] * inv[None, :]
    ang = np.concatenate([ang, ang], axis=-1)
    cos = np.cos(ang).astype(np.float32).T
    sin = np.sin(ang).astype(np.float32).T
    sgn = np.concatenate([-np.ones(half), np.ones(half)]).astype(np.float32)[:, None]
    cosT = np.concatenate([cos, cos], 0)
    sinT = np.concatenate([sin * sgn, sin * sgn], 0)
    p = np.arange(128)[:, None]
    c = np.arange(2560)[None, :]
    dl = c - p - 384
    w = ((dl >= 0) & (dl <= 128)).astype(np.float32) + ((dl >= 0) & (dl <= 512) & (dl % 4 == 0)) \
        + ((dl >= 0) & (dl <= 2048) & (dl % 16 == 0))
    Tb = w.astype(np.float32)
    c2 = np.arange(640)[None, :]
    Tc = ((c2 - p - 384) >= 0).astype(np.float32)
    onehot = np.zeros((128, 8, 128), np.float32)
    for b in range(8):
        onehot[b, b, :] = 1.0
    ident = np.eye(128, dtype=np.float32)
    sw = (p // 64) * 64 + ((p % 64) + 32) % 64
    rperm = (np.arange(128)[:, None] == sw.T).astype(np.float32)
    rperm = rperm.reshape(128, 128)
    blockones = ((np.arange(128)[:, None] // 64) == (np.arange(128)[None, :] // 64)).astype(np.float32)
    ones = np.ones((128, 128), np.float32)
    gmask = np.zeros((128, NT, 4, 8), np.float32)
    for t in range(NT):
        qb = t // 2
        for n in range(8):
            gmask[:, t, :, n] = 0.0 if n < qb else (1e30 if n == qb else -1e30)
    bf = np.concatenate([cosT, sinT, Tb, Tc, onehot.reshape(128, -1), ident, rperm, blockones, ones], axis=1)
    return np.ascontiguousarray(bf, dtype=np.float32), np.ascontiguousarray(gmask.reshape(128, -1))


C_COS, C_SIN, C_TB, C_TC, C_OH, C_ID, C_RP, C_BO, C_ON = 0, 2048, 4096, 6656, 7296, 8320, 8448, 8576, 8704
C_TOT = 8832


def _pvec(inp, l):
    def hd(v, swap=False):
        d = np.arange(128) % 64
        if swap:
            d = (d + 32) % 64
        return v[d]
    cols = []
    for nm in ("q_norm_a", "k_norm_a", "q_norm_b", "k_norm_b"):
        cols += [hd(inp[nm][l]), hd(inp[nm][l], True)]
    cols += [hd(inp["q_norm_m"][l]), hd(inp["k_norm_m"][l])]
    for nm in ("conv_b", "conv_ln_g", "conv_ln_b"):
        cols += [inp[nm][l][0:128], inp[nm][l][128:256]]
    for ct in range(2):
        for j in range(31):
            cols.append(inp["conv_w"][l][j, ct * 128:(ct + 1) * 128])
    on = inp["out_norm"][l]
    for g0 in (0, 256, 768):
        for h in range(4):
            v = on[g0 + 64 * h:g0 + 64 * h + 64]
            cols.append(np.concatenate([v, v]))
    cols += [on[512:640], on[640:768]]
    for j in range(3):
        for ft in range(44):
            cols.append(inp["ffn_conv_w"][l][j, ft * 128:(ft + 1) * 128])
    for ft in range(44):
        cols.append(inp["ffn_conv_b"][l][ft * 128:(ft + 1) * 128])
    out = np.stack(cols, axis=1).astype(np.float32)
    assert out.shape == (128, NV)
    return out


def build(n_seq=2, n_layers=DEPTH, dbg=None):
    nc = bass.Bass("TRN2", target_bir_lowering=False)

    def din(name, shape):
        return nc.dram_tensor(name, list(shape), F32, kind="ExternalInput").ap()

    x_d = din("x", [2, S, D])
    mem_d = din("mem", [2, 256, D])
    w_in_d = din("w_in", [DEPTH, D, 2304])
    w_mem_d = din("w_mem_kv", [DEPTH, D, 512])
    w_co_d = din("w_conv_out", [DEPTH, 256, 256])
    w_out_d = din("w_out", [DEPTH, D, D])
    w_up_d = din("w_up", [DEPTH, D, 2 * DFF])
    w_dn_d = din("w_down", [DEPTH, DFF, D])
    nmix_d = din("norm_mix", [DEPTH, D])
    nffn_d = din("norm_ffn", [DEPTH, D])
    nmem_d = din("mem_norm", [DEPTH, D])
    pvec_d = din("pvec", [DEPTH, 128, NV])
    cst_d = din("cst", [128, C_TOT])
    gmask_d = din("gmask", [128, 512])
    out_d = nc.dram_tensor("out", [2, S, D], F32, kind="ExternalOutput").ap()
    scr_d = nc.dram_tensor("scr", [8, 512], F32, kind="Internal").ap()
    r_scr = [Reg("scr%d" % i) for i in range(8)]
    scr_i = [0]
    dbg_d = None
    if dbg is not None:
        dbg_d = nc.dram_tensor("dbg", [128, 16384], F32, kind="ExternalOutput").ap()

    P = Prog()

    def MM(out, lhsT, rhs, start, stop, reads, writes):
        P.pe(lambda e: e.matmul(out, lhsT, rhs, start=start, stop=stop), reads, writes)

    def ACT(out, in_, func, reads, writes, bias=None, scale=None, accum=None):
        kw = {}
        if bias is not None:
            kw["bias"] = bias
        if scale is not None:
            kw["scale"] = scale
        if accum is not None:
            kw["accum_out"] = accum
        P.act(lambda e: e.activation(out, in_, func, **kw), reads, writes)

    def ACOPY(out, in_, reads, writes):
        P.act(lambda e: e.copy(out, in_), reads, writes)

    def ENG(eng):
        return {"dve": P.dve, "pool": P.pool}[eng]

    def TT(eng, out, in0, in1, op, reads, writes):
        ENG(eng)(lambda e: e.tensor_tensor(out, in0, in1, op), reads, writes)

    def TS(eng, out, in0, s1, s2, op0, op1, reads, writes):
        if op1 is None:
            ENG(eng)(lambda e: e.tensor_scalar(out, in0, s1, None, op0), reads, writes)
        else:
            ENG(eng)(lambda e: e.tensor_scalar(out, in0, s1, s2, op0, op1), reads, writes)

    def STT(eng, out, in0, sc, in1, op0, op1, reads, writes):
        ENG(eng)(lambda e: e.scalar_tensor_tensor(out, in0, sc, in1, op0, op1), reads, writes)

    def RECIP(out, in_, reads, writes):
        P.dve(lambda e: e.reciprocal(out, in_), reads, writes)

    def RED(out, in_, reads, writes):
        P.dve(lambda e: e.tensor_reduce(out, in_, AX.X, ALU.add), reads, writes)

    def CP(eng, out, in_, reads, writes):
        ENG(eng)(lambda e: e.tensor_copy(out, in_), reads, writes)

    def MEMSET(eng, ap, val, writes):
        ENG(eng)(lambda e: e.memset(ap, val), (), writes)

    def DMA(q, out, in_, reads=(), writes=()):
        return P.dma(q, lambda e: e.dma_start(out=out, in_=in_), reads, writes)

    st = contextlib.ExitStack()
    with st:
        def sb(name, shape, dt):
            return st.enter_context(nc.sbuf_tensor(name, shape, dt))

        def ps(name, shape, dt):
            return st.enter_context(nc.psum_tensor(name, shape, dt))

        X = sb("X", [128, NT, D], F32)
        rX = [Reg("x%d" % t) for t in range(NT)]
        pv = sb("pv", [128, DEPTH, NV], F32)
        r_pv = Reg("pv")
        cb = sb("cb", [128, 1536], BF16)
        r_cb = Reg("cb")
        onesf = sb("onesf", [128, 64], F32)
        r_onesf = Reg("onesf")
        gm = sb("gm", [128, 512], F32)
        r_gm = Reg("gm")
        stat = sb("stat", [128, 32], F32)
        r_stat = Reg("stat")
        NB = 57600
        NF = 4700
        arB_t = sb("arB", [128, NB], BF16)
        arF_t = sb("arF", [128, NF], F32)
        AB = Arena(arB_t, 0, NB)
        AFa = Arena(arF_t, 0, NF)
        pb = [ps("pb%d" % i, [128, 512], F32) for i in range(7)]
        r_pb = [Reg("pb%d" % i) for i in range(7)]
        ptr = ps("ptr", [128, 1024], BF16)
        r_ptr = Reg("ptr")

        if dbg is not None:
            tq = sb("dbgbuf", [128, 512], F32)
            r_tq = Reg("dbgbuf")
        ident = cb[:, 0:128]
        rperm = cb[:, 128:256]
        bones = cb[:, 256:384]
        ones = cb[:, 384:512]
        onehot = cb[:, 512:1536]

        DMA("sp", pv[:], pvec_d.rearrange("l p n -> p l n"), writes=[r_pv])
        DMA("pool", cb[:, 0:512], cst_d[:, C_ID:C_ID + 512], writes=[r_cb])
        DMA("pool", cb[:, 512:1536], cst_d[:, C_OH:C_OH + 1024], writes=[r_cb])
        DMA("sp", gm[:], gmask_d, writes=[r_gm])
        MEMSET("dve", onesf[:], 1.0, [r_onesf])
        idf = sb("idf", [128, 128], F32)
        r_idf = Reg("idf")
        DMA("sp", idf[:], cst_d[:, C_ID:C_ID + 128], writes=[r_idf])
        rsT2 = [(sb("rsT%d" % i, [128, 8], F32), Reg("rsT%d" % i)) for i in range(2)]
        hsel = cb[:, 256:384:64]
        epsb = sb("epsb", [128, 1], F32)
        r_eps = Reg("eps")
        MEMSET("dve", epsb[:], EPS, [r_eps])

        def RSQ(dst, src, scale, reads, r_dst, extra_writes=(), ln=False):
            if not ln:
                ACT(dst, src, AF.Sqrt, list(reads) + [r_eps], [r_dst] + list(extra_writes),
                    bias=epsb[0:dst.shape[0], :] if dst.shape[0] != 128 else epsb[:], scale=scale)
                RECIP(dst, dst, [r_dst], [r_dst])
                return
            ACT(dst, src, AF.Ln, list(reads) + [r_eps], [r_dst] + list(extra_writes), bias=epsb[0:dst.shape[0], :] if dst.shape[0] != 128 else epsb[:], scale=scale)
            ACT(dst, dst, AF.Exp, [r_dst], [r_dst], scale=-0.5)

        def rms_to_hT(src_tiles, r_src, gain_row, tmp, hT, r_hT_of, tok_of):
            gB, r_g = tmp["gain"]
            junk, r_junk = tmp["junk"]
            ht = tmp["ht"]
            n_tiles = len(src_tiles)
            DMA("sp", gB, gain_row.partition_broadcast(128), writes=[r_g])
            for t in range(n_tiles):
                ACT(junk, src_tiles[t], AF.Square, [r_src[t]], [r_junk, r_stat], accum=stat[:, t:t + 1])
            sl = stat[:, 0:n_tiles]
            RSQ(sl, sl, 1.0 / D, [r_stat], r_stat)
            for t in range(n_tiles):
                hb, r_hb = ht[t % 2]
                STT("dve", hb, src_tiles[t], stat[:, t:t + 1], gB, ALU.mult, ALU.mult, [r_src[t], r_stat, r_g], [r_hb])
                for c in range(8):
                    P.pe((lambda o_, i_: (lambda e: e.transpose(o_, i_, ident)))(ptr[:, c * 128:(c + 1) * 128], hb[:, c * 128:(c + 1) * 128]),
                         [r_hb, r_cb], [r_ptr])
                o = tok_of(t)
                ACOPY(hT[:, :, o:o + 128], ptr[:, :].rearrange("p (c t) -> p c t", c=8), [r_ptr], [r_hT_of(t)])

        out_ops = []
        dbg_ops = []

        for s in range(n_seq):
            for t in range(NT):
                DMA("sp", X[:, t, :], x_d[s, t * 128:(t + 1) * 128, :], writes=[rX[t]])
            for l in range(n_layers):
                def pcol(c, l=l):
                    return pv[:, l, c:c + 1]
                P.barrier()
                AB.reset()
                AFa.reset()
                qk_ap, _ = AB.alloc(5 * 2 * S, "qk")
                qk = qk_ap.rearrange("p (k c t) -> p k c t", k=5, c=2)
                r_qk = [[[Reg("qk") for j in range(4)] for c in range(2)] for k in range(5)]
                Vp_ap, _ = AB.alloc(NT * 8 * 65, "Vp")
                Vp = Vp_ap.rearrange("p (t h d) -> p t h d", t=NT, h=8)
                r_Vp = [Reg("Vp%d" % t) for t in range(NT)]
                Vm_ap, r_Vm = AB.alloc(2 * 4 * 65, "Vm")
                Vm = Vm_ap.rearrange("p (t h d) -> p t h d", t=2, h=4)
                kmT_ap, r_kmT = AB.alloc(2 * 256, "kmT")
                kmT = kmT_ap.rearrange("p (c t) -> p c t", c=2)
                uT_off = AB.mark()
                uT_ap, _ = AB.alloc(2 * 2080, "uT")
                uT = uT_ap.rearrange("p (c t) -> p c t", c=2)
                r_uT = [[Reg("uT") for j in range(4)] for c in range(2)]
                base_m = AB.mark()
                base_f = AFa.mark()

                MEMSET("pool", Vp[:, :, :, 64:65], 1.0, r_Vp)
                MEMSET("pool", Vm[:, :, :, 64:65], 1.0, [r_Vm])
                MEMSET("pool", uT[:, :, 0:32], 0.0, [r_uT[0][0], r_uT[1][0]])

                memt_ap, _ = AFa.alloc(2 * D, "memt")
                memt = memt_ap.rearrange("p (j d) -> p j d", j=2)
                r_memt = [Reg("memt0"), Reg("memt1")]
                for j in range(2):
                    DMA("sp", memt[:, j, :], mem_d[s, j * 128:(j + 1) * 128, :], writes=[r_memt[j]])
                mhT_ap, r_mhT = AB.alloc(8 * 256, "mhT")
                mhT = mhT_ap.rearrange("p (c t) -> p c t", c=8)
                wm_ap, r_wm = AB.alloc(8 * 512, "wm")
                wm = wm_ap.rearrange("p (c n) -> p c n", c=8)
                DMA("pool", wm, w_mem_d[l].rearrange("(c p) n -> p c n", p=128), writes=[r_wm])
                tmp = dict(gain=AFa.alloc(D, "gain"), junk=AB.alloc(D, "junk"), ht=[AB.alloc(D, "ht") for _ in range(2)])
                rms_to_hT([memt[:, t, :] for t in range(2)], r_memt, nmem_d[l:l + 1, :], tmp, mhT, lambda t: r_mhT, lambda t: t * 128)
                sqm, r_sqm = AB.alloc(256, "sqm")
                rsm, r_rsm = AFa.alloc(256, "rsm")
                for ct in range(2):
                    A_, rA = pb[ct], r_pb[ct]
                    for c in range(8):
                        MM(A_[:, 0:256], wm[:, c, ct * 128:(ct + 1) * 128], mhT[:, c, :], c == 0, c == 7, [r_wm, r_mhT], [rA])
                    ACT(sqm, A_[:, 0:256], AF.Square, [rA], [r_sqm])
                    B_, rB = pb[2 + ct], r_pb[2 + ct]
                    MM(B_[:, 0:256], bones, sqm, True, True, [r_sqm, r_cb], [rB])
                    RSQ(rsm, B_[:, 0:256], 1.0 / 64, [rB], r_rsm)
                    STT("dve", kmT[:, ct, :], A_[:, 0:256], pcol(9), rsm, ALU.mult, ALU.mult, [rA, r_rsm, r_pv], [r_kmT])
                for j in range(2):
                    A_, rA = pb[4 + j], r_pb[4 + j]
                    for c in range(8):
                        MM(A_[:, 0:256], mhT[:, c, j * 128:(j + 1) * 128], wm[:, c, 256:512], c == 0, c == 7, [r_wm, r_mhT], [rA])
                    ACOPY(Vm[:, j, :, 0:64], A_[:, 0:256].rearrange("p (h d) -> p h d", h=4), [rA], [r_Vm])
                P.barrier()
                AB.reset(base_m)
                AFa.reset(base_f)

                cosb, r_cos = AB.alloc(S, "cos")
                sinb, r_sin = AB.alloc(S, "sin")
                DMA("pool", cosb, cst_d[:, C_COS:C_COS + S], writes=[r_cos])
                DMA("pool", sinb, cst_d[:, C_SIN:C_SIN + S], writes=[r_sin])
                hT_ap, _ = AB.alloc(8 * 1024, "hT")
                hT = hT_ap.rearrange("p (c t) -> p c t", c=8)
                r_hT = [Reg("hT0"), Reg("hT1")]
                wst = []
                for i in range(2):
                    a, r = AB.alloc(8 * 256, "wst")
                    wst.append((a.rearrange("p (c n) -> p c n", c=8), r))
                sqb = [AB.alloc(512, "sq") for _ in range(2)]
                qbb = [AB.alloc(512, "qb") for _ in range(2)]
                sigb = {(ct, jc): AB.alloc(512, "sig") for ct in range(2) for jc in range(2)}
                rsb = [AFa.alloc(512, "rs") for _ in range(2)]
                t1, r_t1 = AFa.alloc(512, "t1")
                t2, r_t2 = AFa.alloc(512, "t2")
                tmp = dict(gain=AFa.alloc(D, "gain"), junk=AB.alloc(D, "junk"), ht=[AB.alloc(D, "ht") for _ in range(2)])
                w_l = w_in_d[l].rearrange("(c p) n -> p c n", p=128)
                strips = [(0, "qk", 0), (256, "qk", 1), (768, "qk", 2), (1024, "qk", 3), (2048, "qm", 4),
                          (1792, "cg", None), (1536, "cv", None)]
                si = 0
                all_strips = []
                for hf in range(2):
                    all_strips.append((hf, 512, "v", 0))
                    all_strips.append((hf, 1280, "v", 1))
                    for (col0, kind, qi) in strips:
                        all_strips.append((hf, col0, kind, qi))

                def load_strip(n):
                    if n >= len(all_strips):
                        return
                    wS_, r_wS_ = wst[n % 2]
                    c0 = all_strips[n][1]
                    DMA("pool", wS_, w_l[:, :, c0:c0 + 256], writes=[r_wS_])
                load_strip(0)
                gi = 0
                for hf in range(2):
                    rms_to_hT([X[:, hf * 8 + t, :] for t in range(8)], rX[hf * 8:hf * 8 + 8], nmix_d[l:l + 1, :], tmp, hT,
                              lambda t: r_hT[t // 4], lambda t: t * 128)
                    tiles = []
                    for sn in range(hf * 9, hf * 9 + 9):
                        _, col0, kind, qi = all_strips[sn]
                        wS, r_wS = wst[sn % 2]
                        if kind == "v":
                            load_strip(sn + 1)
                            for t in range(8):
                                A_, rA = pb[5 + (t % 2)], r_pb[5 + (t % 2)]
                                for c in range(8):
                                    MM(A_[:, 0:256], hT[:, c, t * 128:(t + 1) * 128], wS[:, c, :], c == 0, c == 7, [r_wS, r_hT[t // 4]], [rA])
                                tt = hf * 8 + t
                                ACOPY(Vp[:, tt, qi * 4:qi * 4 + 4, 0:64], A_[:, 0:256].rearrange("p (h d) -> p h d", h=4), [rA], [r_Vp[tt]])
                            continue
                        for ct in range(2):
                            for jc in range(2):
                                tiles.append(dict(sn=sn, first=(ct == 0 and jc == 0), wS=wS, r_wS=r_wS, kind=kind, qi=qi, ct=ct, jc=jc, gi=gi))
                                gi += 1

                    def stA(u):
                        wS, r_wS, kind, ct, jc, k = u["wS"], u["r_wS"], u["kind"], u["ct"], u["jc"], u["gi"]
                        if u["first"]:
                            load_strip(u["sn"] + 1)
                        tk = jc * 512
                        g0 = (hf * 2 + jc) * 512
                        A_, rA = pb[k % 3], r_pb[k % 3]
                        for c in range(8):
                            MM(A_[:, :], wS[:, c, ct * 128:(ct + 1) * 128], hT[:, c, tk:tk + 512], c == 0, c == 7, [r_wS, r_hT[jc]], [rA])
                        if kind in ("qk", "qm"):
                            sq, r_sq = sqb[k % 2]
                            ACT(sq, A_[:, :], AF.Square, [rA], [r_sq])
                            if kind == "qk":
                                qb_, r_qb = qbb[k % 2]
                                ACOPY(qb_, A_[:, :], [rA], [r_qb])
                        elif kind == "cg":
                            sg, r_sg = sigb[(ct, jc)]
                            ACT(sg, A_[:, :], AF.Sigmoid, [rA], [r_sg])
                        else:
                            sg, r_sg = sigb[(ct, jc)]
                            TT("dve", uT[:, ct, 32 + g0:32 + g0 + 512], A_[:, :], sg, ALU.mult, [rA, r_sg], [r_uT[ct][hf * 2 + jc]])

                    def stB(u):
                        kind, k = u["kind"], u["gi"]
                        if kind not in ("qk", "qm"):
                            return
                        sq, r_sq = sqb[k % 2]
                        rs, r_rs = rsb[k % 2]
                        B_, rB = pb[3 + (k % 2)], r_pb[3 + (k % 2)]
                        if QK_COMPACT:
                            rsT, r_rsT = rsT2[k % 2]
                            for sub in range(4):
                                MM(B_[:, sub * 2:sub * 2 + 2], sq[:, sub * 128:(sub + 1) * 128], hsel, True, True, [r_sq, r_cb], [rB])
                        else:
                            MM(B_[:, :], bones, sq, True, True, [r_sq, r_cb], [rB])
                        if kind == "qk":
                            qb_, r_qb = qbb[k % 2]
                            C_, rC = pb[5 + (k % 2)], r_pb[5 + (k % 2)]
                            MM(C_[:, :], rperm, qb_, True, True, [r_qb, r_cb], [rC])
                        if QK_COMPACT:
                            ACT(rsT[:], B_[:, 0:8], AF.Sqrt, [rB, r_eps], [r_rsT], bias=epsb[:], scale=1.0 / 64)
                        elif QK_LN:
                            ACT(rs, B_[:, :], AF.Ln, [rB, r_eps], [r_rs], bias=epsb[:], scale=1.0 / 64)
                            ACT(rs, rs, AF.Exp, [r_rs], [r_rs], scale=-0.5)
                        else:
                            ACT(rs, B_[:, :], AF.Sqrt, [rB, r_eps], [r_rs], bias=epsb[:], scale=1.0 / 64)

                    def stC(u):
                        kind, qi, ct, jc, k = u["kind"], u["qi"], u["ct"], u["jc"], u["gi"]
                        if kind not in ("qk", "qm"):
                            return
                        j = hf * 2 + jc
                        g0 = j * 512
                        A_, rA = pb[k % 3], r_pb[k % 3]
                        rs, r_rs = rsb[k % 2]
                        if QK_COMPACT:
                            rsT, r_rsT = rsT2[k % 2]
                            B_, rB = pb[3 + (k % 2)], r_pb[3 + (k % 2)]
                            RECIP(rsT[:], rsT[:], [r_rsT], [r_rsT])
                            CP("dve", rs.rearrange("p (a d) -> p a d", d=64), bview(rsT[:, 0:8], 64), [r_rsT], [r_rs])
                            for sub in range(4):
                                MM(B_[:, sub * 128:(sub + 1) * 128], rs[:, sub * 128:(sub + 1) * 128], idf[:], True, True, [r_rs, r_idf], [rB])
                            rs, r_rs = B_[:, :], rB
                        elif not QK_LN:
                            RECIP(rs, rs, [r_rs], [r_rs])
                        dst = qk[:, qi, ct, g0:g0 + 512]
                        r_dst = r_qk[qi][ct][j]
                        if kind == "qm":
                            if QK_COMPACT:
                                ACT(t1, A_[:, :], AF.Copy, [rA, r_pv], [r_t1], scale=pcol(8))
                                TT("dve", dst, t1, rs, ALU.mult, [r_t1, r_rs], [r_dst])
                            else:
                                STT("dve", dst, A_[:, :], pcol(8), rs, ALU.mult, ALU.mult, [rA, r_rs, r_pv], [r_dst])
                        else:
                            C_, rC = pb[5 + (k % 2)], r_pb[5 + (k % 2)]
                            gc = 2 * qi
                            STT("dve", t1, A_[:, :], pcol(gc), cosb[:, g0:g0 + 512], ALU.mult, ALU.mult, [rA, r_cos, r_pv], [r_t1])
                            STT("dve", t2, C_[:, :], pcol(gc + 1), sinb[:, g0:g0 + 512], ALU.mult, ALU.mult, [rC, r_sin, r_pv], [r_t2])
                            TT("dve", t1, t1, t2, ALU.add, [r_t1, r_t2], [r_t1])
                            TT("dve", dst, t1, rs, ALU.mult, [r_t1, r_rs], [r_dst])

                    n_u = len(tiles)
                    for i in range(n_u + 2):
                        if i < n_u:
                            stA(tiles[i])
                        if 0 <= i - 1 < n_u:
                            stB(tiles[i - 1])
                        if 0 <= i - 2 < n_u:
                            stC(tiles[i - 2])
                if dbg == "qk":
                    for k in range(4):
                        for c in range(2):
                            CP("dve", tq[:], qk[:, k, c, 0:512], r_qk[k][c], [r_tq])
                            dbg_ops.append(DMA("sp", dbg_d[:, (k * 2 + c) * 512:(k * 2 + c + 1) * 512], tq[:], reads=[r_tq]))
                P.barrier()
                AB.reset(base_m)
                AFa.reset(base_f)

                ocT_ap, _ = AB.alloc(2 * S, "ocT")
                ocT = ocT_ap.rearrange("p (c t) -> p c t", c=2)
                r_ocT = [[Reg("ocT") for j in range(4)] for c in range(2)]
                keep_m = AB.mark()
                dg_ap, r_dg = AB.alloc(2 * 31 * 128, "dg")
                dg = dg_ap.rearrange("p (c j n) -> p c j n", c=2, j=31)
                wco_ap, r_wco = AB.alloc(2 * 256, "wco")
                wco = wco_ap.rearrange("p (c n) -> p c n", c=2)
                DMA("pool", wco, w_co_d[l].rearrange("(c p) n -> p c n", p=128), writes=[r_wco])
                for ct in range(2):
                    for j in range(31):
                        TS("dve", dg[:, ct, j, :], ident, pcol(16 + ct * 31 + j), None, ALU.mult, None, [r_cb, r_pv], [r_dg])
                yf = [AFa.alloc(512, "yf") for _ in range(2)]
                ybf = [AB.alloc(512, "ybf") for _ in range(2)]
                ysq = [AB.alloc(512, "ysq") for _ in range(2)]
                zs = [AB.alloc(512, "zs") for _ in range(2)]
                mean, r_mean = AFa.alloc(512, "mean")
                msq, r_msq = AFa.alloc(512, "msq")
                rstd, r_rstd = AFa.alloc(512, "rstd")
                dd, r_dd = AFa.alloc(512, "dd")
                for j in range(4):
                    g0 = j * 512
                    for ct in range(2):
                        A_, rA = pb[ct], r_pb[ct]
                        rds = [r_uT[ct][j], r_dg] + ([r_uT[ct][j - 1]] if j > 0 else [])
                        for tp in range(31):
                            MM(A_[:, :], dg[:, ct, tp, :], uT[:, ct, 2 + tp + g0:2 + tp + g0 + 512], tp == 0, tp == 30, rds, [rA])
                        ACT(yf[ct][0], A_[:, :], AF.Identity, [rA, r_pv], [yf[ct][1]], bias=pcol(10 + ct))
                        ACT(ysq[ct][0], A_[:, :], AF.Square, [rA, r_pv], [ysq[ct][1]], bias=pcol(10 + ct))
                        CP("dve", ybf[ct][0], yf[ct][0], [yf[ct][1]], [ybf[ct][1]])
                    for ct in range(2):
                        MM(pb[2][:, :], ones, ybf[ct][0], ct == 0, ct == 1, [ybf[ct][1], r_cb], [r_pb[2]])
                    for ct in range(2):
                        MM(pb[3][:, :], ones, ysq[ct][0], ct == 0, ct == 1, [ysq[ct][1], r_cb], [r_pb[3]])
                    TS("dve", mean, pb[2][:, :], 1.0 / 256, None, ALU.mult, None, [r_pb[2]], [r_mean])
                    TT("dve", msq, mean, mean, ALU.mult, [r_mean], [r_msq])
                    STT("dve", rstd, pb[3][:, :], 1.0 / 256, msq, ALU.mult, ALU.subtract, [r_pb[3], r_msq], [r_rstd])
                    RSQ(rstd, rstd, 1.0, [r_rstd], r_rstd)
                    for ct in range(2):
                        TT("dve", dd, yf[ct][0], mean, ALU.subtract, [yf[ct][1], r_mean], [r_dd])
                        TT("dve", dd, dd, rstd, ALU.mult, [r_dd, r_rstd], [r_dd])
                        TS("dve", dd, dd, pcol(12 + ct), pcol(14 + ct), ALU.mult, ALU.add, [r_dd, r_pv], [r_dd])
                        ACT(zs[ct][0], dd, AF.Silu, [r_dd], [zs[ct][1]])
                    for c2 in range(2):
                        O_, rO = pb[4 + c2], r_pb[4 + c2]
                        for ct in range(2):
                            MM(O_[:, :], wco[:, ct, c2 * 128:(c2 + 1) * 128], zs[ct][0], ct == 0, ct == 1, [zs[ct][1], r_wco], [rO])
                        ACOPY(ocT[:, c2, g0:g0 + 512], O_[:, :], [rO], [r_ocT[c2][j]])
                if dbg == "oc":
                    for c in range(2):
                        for j in range(4):
                            CP("dve", tq[:], ocT[:, c, j * 512:(j + 1) * 512], [r_ocT[c][j]], [r_tq])
                            dbg_ops.append(DMA("sp", dbg_d[:, (c * 4 + j) * 512:(c * 4 + j + 1) * 512], tq[:], reads=[r_tq]))
                P.barrier()
                AB.reset(keep_m)
                AFa.reset(base_f)
                AU = Arena(arB_t, uT_off, uT_off + 2 * 2080)

                Tb, r_Tb = AB.alloc(2560, "Tb")
                Tc, r_Tc = AB.alloc(640, "Tc")
                DMA("pool", Tb, cst_d[:, C_TB:C_TB + 2560], writes=[r_Tb])
                DMA("pool", Tc, cst_d[:, C_TC:C_TC + 640], writes=[r_Tc])
                negm_ap, r_negm = AB.alloc(NT * 4 * 8, "negm")
                negm = negm_ap.rearrange("p (t h n) -> p t h n", t=NT, h=4)
                ksum, r_ksum = AFa.alloc(16, "ksum")
                kmean_ap, r_kmean = AB.alloc(16, "kmean")
                kmean = kmean_ap.rearrange("p (c n) -> p c n", c=2)
                for c in range(2):
                    RED(ksum[:, c * 8:(c + 1) * 8], qk[:, 1, c, :].rearrange("p (n k) -> p n k", n=8), r_qk[1][c], [r_ksum])
                TS("dve", kmean_ap, ksum, 1.0 / 256, None, ALU.mult, None, [r_ksum], [r_kmean])
                G_, rG = pb[0], r_pb[0]
                for t in range(NT):
                    for h in range(4):
                        r0 = (h % 2) * 64
                        gcol = (t * 4 + h) * 8
                        MM(G_[:, gcol:gcol + 8], qk[r0:r0 + 64, 0, h // 2, t * 128:(t + 1) * 128], kmean[r0:r0 + 64, h // 2, :], True, True,
                           [r_qk[0][h // 2][t // 4], r_kmean], [rG])
                gate, r_gate = AFa.alloc(512, "gate")
                TT("dve", gate, G_[:, :], gm[:], ALU.add, [rG, r_gm], [r_gate])
                cmpb, r_cmp = AFa.alloc(2048, "cmp")
                rank, r_rank = AFa.alloc(512, "rank")
                for hh in range(2):
                    gsl = gate[:, hh * 256:(hh + 1) * 256]
                    cmp4 = cmpb.rearrange("p (g n m) -> p g n m", g=32, n=8)
                    TT("dve", cmp4, bview_mid(gsl, 32, 8), bview_in(gsl, 32, 8), ALU.is_gt, [r_gate], [r_cmp])
                    RED(rank[:, hh * 256:(hh + 1) * 256], cmpb.rearrange("p (g m) -> p g m", m=8), [r_cmp], [r_rank])
                TS("dve", negm_ap, rank, 4.0, NEG, ALU.is_ge, ALU.mult, [r_rank], [r_negm])
                if dbg == "gate":
                    dbg_ops.append(DMA("sp", dbg_d[:, 0:512], gate, reads=[r_gate]))
                    dbg_ops.append(DMA("sp", dbg_d[:, 512:1024], rank, reads=[r_rank]))

                AFa.reset(base_f)
                mixS_ap, _ = AB.alloc(8 * 512, "mixS")
                mixS = mixS_ap.rearrange("p (k t) -> p k t", k=8)
                r_mix = [Reg("mix%d" % k) for k in range(8)]
                mixO_ap, _ = AB.alloc(6 * 512, "mixO")
                mixO = mixO_ap.rearrange("p (k t) -> p k t", k=6)
                r_mixO = [Reg("mixO%d" % k) for k in range(6)]
                PT = [AU.alloc(512, "PT") for _ in range(3)] + [AB.alloc(512, "PT") for _ in range(7)]
                negT = [AU.alloc(512, "negT") for _ in range(2)]
                sqh = [AU.alloc(512, "sqh") for _ in range(3)] + [AB.alloc(512, "sqh")]
                wo_ap, _ = AB.alloc(8 * 512, "wo")
                wo = wo_ap.rearrange("p (k n) -> p k n", k=8)
                r_wo = [Reg("wo%d" % k) for k in range(8)]
                onh = [AFa.alloc(512, "onh") for _ in range(4)]
                rden2 = [AFa.alloc(512, "rden") for _ in range(2)]
                bcs2 = [AFa.alloc(512, "bcs") for _ in range(2)]
                rsg, r_rsg = AFa.alloc(512, "rsg")
                rows = [0, 128, 256, 384, 768, 896, 512, 640]
                epi = [0]
                pti = 0
                sti = 0
                pending = []

                pending_head = []

                def flush_head():
                    fs = list(pending_head)
                    del pending_head[:]
                    for f_ in fs:
                        f_()

                def flush_pending():
                    flush_head()
                    fs = list(pending)
                    del pending[:]
                    for f_ in fs:
                        f_()
                for qc in range(4):
                    q0 = qc * 512
                    for grp in range(3):
                        stream = []
                        if grp == 2:
                            tiles = [(kt, q0, 512) for kt in range(2)]
                        elif grp == 0:
                            tiles = []
                            for kt in range(4 * qc + 4):
                                ql = max(q0, 256 * (kt // 2))
                                tiles.append((kt, ql, q0 + 512 - ql))
                        else:
                            tiles = []
                            for kt in range(4 * qc + 4):
                                ql = max(q0, 128 * kt)
                                tiles.append((kt, ql, q0 + 512 - ql))
                        for p_ in range(2):
                            for ti, (kt, ql, N) in enumerate(tiles):
                                stream.append(dict(p=p_, ti=ti, nt=len(tiles), kt=kt, ql=ql, N=N))
                        SBK = [(pb[0][:, :], r_pb[0]), (pb[1][:, :], r_pb[1]), (pb[4][:, :], r_pb[4]), (pb[5][:, :], r_pb[5]),
                               (pb[6][:, :], r_pb[6]), (ptr[:, :].bitcast(F32), r_ptr)]

                        def stage1(it):
                            nonlocal sti, pti
                            p_, ti, kt, ql, N = it["p"], it["ti"], it["kt"], it["ql"], it["N"]
                            co = ql - q0
                            it["pt"] = []
                            sbk = []
                            for hh in range(2):
                                h = 2 * p_ + hh
                                nT, r_nT = negT[hh]
                                if grp == 0 and ti == 0:
                                    for t4 in range(4):
                                        t = qc * 4 + t4
                                        P.pe((lambda o_, i_: (lambda e: e.transpose(o_, i_, ident)))(ptr[0:8, t4 * 128:(t4 + 1) * 128], negm[:, t, h, :]),
                                             [r_negm, r_cb], [r_ptr])
                                    ACOPY(nT[0:8, :], ptr[0:8, 0:512], [r_ptr], [r_nT])
                                sbk.append(SBK[(sti % 3) * 2 + hh])
                                it["pt"].append(PT[pti % 10])
                                pti += 1
                            sti += 1
                            th = p_
                            for hh in range(2):
                                r0 = hh * 64
                                S_, rS = sbk[hh]
                                if grp == 2:
                                    MM(S_[:, 0:512], kmT[r0:r0 + 64, th, kt * 128:(kt + 1) * 128], qk[r0:r0 + 64, 4, th, q0:q0 + 512], True, True,
                                       [r_kmT, r_qk[4][th][qc]], [rS])
                                else:
                                    qi, ki = (0, 1) if grp == 0 else (2, 3)
                                    MM(S_[:, 0:N], qk[r0:r0 + 64, ki, th, kt * 128:(kt + 1) * 128], qk[r0:r0 + 64, qi, th, ql:ql + N], True, grp == 1,
                                       [r_qk[ki][th][kt // 4], r_qk[qi][th][qc]], [rS])
                            if grp == 0:
                                kb = kt // 2
                                for hh in range(2):
                                    S_, rS = sbk[hh]
                                    nT, r_nT = negT[hh]
                                    MM(S_[:, 0:N], onehot[0:8, kb * 128:(kb + 1) * 128], nT[0:8, co:co + N], False, True, [r_cb, r_nT], [rS])
                            for hh in range(2):
                                S_, rS = sbk[hh]
                                pt, r_pt = it["pt"][hh]
                                ACT(pt[:, 0:N], S_[:, 0:N], AF.Exp, [rS], [r_pt], scale=0.125)
                            for hh in range(2):
                                pt, r_pt = it["pt"][hh]
                                if grp == 0 and (kt // 2) >= 2 * qc:
                                    off = ql - 128 * kt + 384
                                    TT("dve", pt[:, 0:256], pt[:, 0:256], Tc[:, off:off + 256], ALU.mult, [r_pt, r_Tc], [r_pt])
                                if grp == 1:
                                    off = ql - 128 * kt + 384
                                    TT("dve", pt[:, 0:N], pt[:, 0:N], Tb[:, off:off + N], ALU.mult, [r_pt, r_Tb], [r_pt])

                        def stage2(it):
                            p_, ti, nt, kt, ql, N = it["p"], it["ti"], it["nt"], it["kt"], it["ql"], it["N"]
                            co = ql - q0
                            for hh in range(2):
                                h = 2 * p_ + hh
                                pt, r_pt = it["pt"][hh]
                                O_, rO = pb[2 + hh], r_pb[2 + hh]
                                if grp == 2:
                                    lhs, rl = Vm[:, kt, h, :], r_Vm
                                else:
                                    lhs, rl = Vp[:, kt, grp * 4 + h, :], r_Vp[kt]
                                MM(O_[0:65, co:co + N], lhs, pt[:, 0:N], ti == 0, ti == nt - 1, [rl, r_pt], [rO])
                            if pending_head and ti == min(3, nt - 1):
                                flush_head()
                            if ti == nt - 1:
                                if p_ == 0:
                                    flush_pending()
                                for hh in range(2):
                                    h = 2 * p_ + hh
                                    O_, rO = pb[2 + hh], r_pb[2 + hh]
                                    on, r_on = onh[h]
                                    sq, r_sq = sqh[h]
                                    rden, r_rden = rden2[hh]
                                    bcs, r_bcs = bcs2[hh]
                                    ACOPY(on[0:65, :], O_[0:65, :], [rO], [r_on])
                                    RECIP(rden[64:65, :], on[64:65, :], [r_on], [r_rden])
                                    si_ = scr_i[0] % 8
                                    scr_i[0] += 1
                                    DMA("sp", scr_d[si_:si_ + 1, :], rden[64:65, :], reads=[r_rden], writes=[r_scr[si_]])
                                    DMA("sp", bcs[0:64, :], scr_d[si_:si_ + 1, :].partition_broadcast(64), reads=[r_scr[si_]], writes=[r_bcs])

                                    def part1b(on=on, r_on=r_on, sq=sq, r_sq=r_sq, bcs=bcs, r_bcs=r_bcs):
                                        TT("dve", on[0:64, :], on[0:64, :], bcs[0:64, :], ALU.mult, [r_on, r_bcs], [r_on])
                                        ACT(sq[0:64, :], on[0:64, :], AF.Square, [r_on], [r_sq])
                                    pending_head.append(part1b)

                        n_it = len(stream)
                        for i_ in range(n_it + 2):
                            if i_ < n_it:
                                stage1(stream[i_])
                            if 0 <= i_ - 2 < n_it:
                                stage2(stream[i_ - 2])
                        def part2(grp=grp, l=l):
                            for h in range(4):
                                MM(pb[5][0:64, :], ones[0:64, 0:64], sqh[h][0][0:64, :], h == 0, h == 3, [sqh[h][1], r_cb], [r_pb[5]])
                            RSQ(rsg[0:64, :], pb[5][0:64, :], 1.0 / 256, [r_pb[5]], r_rsg)
                            for h in range(4):
                                k = grp * 4 + h
                                c = grp * 2 + h // 2
                                if h % 2 == 0:
                                    STT("dve", mixS[0:64, c, :], onh[h][0][0:64, :], pv[0:64, l, 78 + k:79 + k], rsg[0:64, :], ALU.mult, ALU.mult,
                                        [onh[h][1], r_rsg, r_pv], [r_mix[c]])
                                else:
                                    o_i = grp * 2 + h // 2
                                    STT("dve", mixO[0:64, o_i, :], onh[h][0][0:64, :], pv[0:64, l, 78 + k:79 + k], rsg[0:64, :], ALU.mult, ALU.mult,
                                        [onh[h][1], r_rsg, r_pv], [r_mixO[o_i]])
                                    DMA("sp", mixS[64:128, c, :], mixO[0:64, o_i, :], reads=[r_mixO[o_i]], writes=[r_mix[c]])
                        pending.append(part2)
                    def cgroup(qc=qc, q0=q0):
                        for ct in range(2):
                            ACT(sqh[ct][0], ocT[:, ct, q0:q0 + 512], AF.Square, [r_ocT[ct][qc]], [sqh[ct][1]])
                        for ct in range(2):
                            MM(pb[5][:, :], ones, sqh[ct][0], ct == 0, ct == 1, [sqh[ct][1], r_cb], [r_pb[5]])
                        RSQ(rsg, pb[5][:, :], 1.0 / 256, [r_pb[5]], r_rsg)
                        for ct in range(2):
                            STT("dve", mixS[:, 6 + ct, :], ocT[:, ct, q0:q0 + 512], pcol(90 + ct), rsg, ALU.mult, ALU.mult,
                                [r_ocT[ct][qc], r_rsg, r_pv], [r_mix[6 + ct]])

                    def wout(qc=qc, l=l):
                        for ch in range(2):
                            for k in range(8):
                                DMA("pool", wo[:, k, :], w_out_d[l, rows[k]:rows[k] + 128, ch * 512:(ch + 1) * 512], writes=[r_wo[k]])
                            for t4 in range(4):
                                t = qc * 4 + t4
                                W_, rW = pb[5 + (t4 % 2)], r_pb[5 + (t4 % 2)]
                                for k in range(8):
                                    MM(W_[:, :], mixS[:, k, t4 * 128:(t4 + 1) * 128], wo[:, k, :], k == 0, k == 7, [r_mix[k], r_wo[k]], [rW])
                                xs = X[:, t, ch * 512:(ch + 1) * 512]
                                TT("dve", xs, xs, W_[:, :], ALU.add, [rW, rX[t]], [rX[t]])
                    pending.append(cgroup)
                    pending.append(wout)
                flush_pending()

                if dbg == "mix":
                    for t in range(NT):
                        dbg_ops.append(DMA("sp", dbg_d[:, t * 1024:(t + 1) * 1024], X[:, t, :], reads=[rX[t]]))

                P.barrier()
                AB.reset()
                AFa.reset()
                hT_ap, _ = AB.alloc(8 * S, "hTf")
                hT = hT_ap.rearrange("p (c t) -> p c t", c=8)
                r_hT = [Reg("hTf%d" % j) for j in range(4)]
                tmp = dict(gain=AFa.alloc(D, "gain"), junk=AB.alloc(D, "junk"), ht=[AB.alloc(D, "ht") for _ in range(2)])
                rms_to_hT([X[:, t, :] for t in range(NT)], rX, nffn_d[l:l + 1, :], tmp, hT, lambda t: r_hT[t // 4], lambda t: t * 128)
                gT_ap, _ = AB.alloc(8 * S, "gT")
                gT = gT_ap.rearrange("p (c t) -> p c t", c=8)
                r_gT = [[Reg("gT") for j in range(4)] for jj in range(8)]
                wd_ap, r_wd = AB.alloc(8 * D, "wd")
                wd = wd_ap.rearrange("p (c n) -> p c n", c=8)
                wu = []
                for i in range(2):
                    a, r = AB.alloc(8 * 256, "wu")
                    wu.append((a.rearrange("p (c k n) -> p c k n", c=8, k=2), r))
                ub = [[AFa.alloc(514, "ub") for _ in range(3)] for _ in range(2)]
                yb = [[AB.alloc_f32(512, "yb") for _ in range(3)] for _ in range(2)]
                sgl2 = [AB.alloc_f32(512, "sgl") for _ in range(2)]
                wu_l = w_up_d[l].rearrange("(c p) n -> p c n", p=128)
                f_groups = [(0, 8), (8, 8), (16, 6)]
                fi = 0
                ui = 0

                def load_wu(n):
                    if n >= NFT:
                        return
                    wU_, r_wU_ = wu[n % 2]
                    DMA("pool", wU_[:, :, 0, :], wu_l[:, :, n * 128:(n + 1) * 128], writes=[r_wU_])
                    DMA("pool", wU_[:, :, 1, :], wu_l[:, :, DFF + n * 128:DFF + (n + 1) * 128], writes=[r_wU_])
                load_wu(0)
                for (f0, nf) in f_groups:
                    DMA("pool", wd[:, 0:nf, :], w_dn_d[l, f0 * 128:(f0 + nf) * 128, :].rearrange("(c p) n -> p c n", p=128), writes=[r_wd])
                    units = []
                    for jj in range(nf):
                        wU, r_wU = wu[fi % 2]
                        fi += 1
                        for j in range(4):
                            units.append(dict(jj=jj, ft=f0 + jj, j=j, ui=ui, wU=wU, r_wU=r_wU))
                            ui += 1

                    def stA(u):
                        jj, ft, j, k, wU, r_wU = u["jj"], u["ft"], u["j"], u["ui"], u["wU"], u["r_wU"]
                        if j == 1:
                            load_wu(ft + 1)
                        g0 = j * 512
                        for kind in range(2):
                            if k % 3 == 2:
                                A_, rA = (pb[6][:, :], r_pb[6]) if kind == 0 else (ptr[:, :].bitcast(F32), r_ptr)
                            else:
                                A_, rA = pb[kind * 2 + (k % 3)][:, :], r_pb[kind * 2 + (k % 3)]
                            for c in range(8):
                                MM(A_, wU[:, c, kind, :], hT[:, c, g0:g0 + 512], c == 0, c == 7, [r_wU, r_hT[j]], [rA])
                            u_, r_u = ub[kind][k % 3]
                            up_, r_up = ub[kind][(k + 2) % 3]
                            y_, r_y = yb[kind][k % 3]
                            ftk = ft + 22 * kind
                            if j == 0:
                                MEMSET("pool", u_[:, 0:2], 0.0, [r_u])
                            else:
                                CP("pool", u_[:, 0:2], up_[:, 512:514], [r_up], [r_u])
                            ACOPY(u_[:, 2:514], A_, [rA], [r_u])
                            ACT(y_, A_, AF.Identity, [rA, r_pv], [r_y], bias=pcol(224 + ftk), scale=pcol(92 + 88 + ftk))

                    def stB(u):
                        ft, k = u["ft"], u["ui"]
                        for kind in range(2):
                            u_, r_u = ub[kind][k % 3]
                            y_, r_y = yb[kind][k % 3]
                            ftk = ft + 22 * kind
                            STT("dve", y_, u_[:, 1:513], pcol(92 + 44 + ftk), y_, ALU.mult, ALU.add, [r_u, r_y, r_pv], [r_y])
                            STT("dve", y_, u_[:, 0:512], pcol(92 + ftk), y_, ALU.mult, ALU.add, [r_u, r_y, r_pv], [r_y])

                    def stC(u):
                        jj, j, k = u["jj"], u["j"], u["ui"]
                        g0 = j * 512
                        sg, r_sg = sgl2[k % 2]
                        yg, r_yg = yb[0][k % 3]
                        yv, r_yv = yb[1][k % 3]
                        ACT(sg, yg, AF.Silu, [r_yg], [r_sg])
                        TT("dve", gT[:, jj, g0:g0 + 512], sg, yv, ALU.mult, [r_sg, r_yv], [r_gT[jj][j]])

                    n_u = len(units)
                    for i in range(n_u + 2):
                        if i < n_u:
                            stA(units[i])
                        if 0 <= i - 1 < n_u:
                            stB(units[i - 1])
                        if 0 <= i - 2 < n_u:
                            stC(units[i - 2])
                    for t in range(NT):
                        for ch in range(2):
                            W_, rW = pb[4 + ((t * 2 + ch) % 2)], r_pb[4 + ((t * 2 + ch) % 2)]
                            for jj in range(nf):
                                MM(W_[:, :], gT[:, jj, t * 128:(t + 1) * 128], wd[:, jj, ch * 512:(ch + 1) * 512], jj == 0, jj == nf - 1,
                                   [r_gT[jj][t // 4], r_wd], [rW])
                            xs = X[:, t, ch * 512:(ch + 1) * 512]
                            TT("dve", xs, xs, W_[:, :], ALU.add, [rW, rX[t]], [rX[t]])
            for t in range(NT):
                out_ops.append(DMA("sp", out_d[s, t * 128:(t + 1) * 128, :], X[:, t, :], reads=[rX[t]]))
        P.emit(nc, final_wait_ops=out_ops + dbg_ops)
    return nc


_CACHE = {}


def _prep(inputs):
    inp = {k: np.asarray(v) for k, v in inputs.items()}
    cst, gmask = _consts()
    pvec = np.stack([_pvec(inp, l) for l in range(DEPTH)], 0)
    shared = dict(
        w_in=np.ascontiguousarray(inp["w_in"], np.float32), w_mem_kv=np.ascontiguousarray(inp["w_mem_kv"], np.float32),
        w_conv_out=np.ascontiguousarray(inp["w_conv_out"], np.float32), w_out=np.ascontiguousarray(inp["w_out"], np.float32),
        w_up=np.ascontiguousarray(inp["w_up"], np.float32), w_down=np.ascontiguousarray(inp["w_down"], np.float32),
        norm_mix=np.ascontiguousarray(inp["norm_mix"], np.float32), norm_ffn=np.ascontiguousarray(inp["norm_ffn"], np.float32),
        mem_norm=np.ascontiguousarray(inp["mem_norm"], np.float32), pvec=pvec, cst=cst, gmask=gmask)
    in_maps = []
    for c in range(N_CORES):
        m = dict(shared)
        m["x"] = np.ascontiguousarray(inp["x"][2 * c:2 * c + 2], np.float32)
        m["mem"] = np.ascontiguousarray(inp["mem"][2 * c:2 * c + 2], np.float32)
        in_maps.append(m)
    return in_maps


def kernel(**inputs):
    in_maps = _prep(inputs)
    if "nc" not in _CACHE:
        _CACHE["nc"] = build()
    res = run_bass_kernel_spmd(_CACHE["nc"], in_maps, core_ids=list(range(N_CORES)))
    return np.concatenate([r["out"] for r in res.results], axis=0).astype(np.float32)
```

```python
import contextlib
import numpy as np
import concourse.bass as bass
import concourse.mybir as mybir
from concourse.bass_utils import run_bass_kernel_spmd

F32 = mybir.dt.float32
BF16 = mybir.dt.bfloat16
ALU = mybir.AluOpType
AF = mybir.ActivationFunctionType
AX = mybir.AxisListType

N_CORES = 8
DEPTH = 2
S = 2048
D = 1024
NT = 16
DFF = 2816
NFT = 22
EPS = 1e-6
N_DMA_SEMS = 40
NV = 268
NEG = -30000.0
import os
ATT_LA = int(os.environ.get('ATT_LA', '1'))
RSQ_OLD = int(os.environ.get('RSQ_OLD', '1'))
QK_LN = int(os.environ.get('QK_LN', '0'))
QK_COMPACT = int(os.environ.get('QK_COMPACT', '1'))


class Reg:
    __slots__ = ("name", "lw", "readers")

    def __init__(self, name):
        self.name = name
        self.lw = None
        self.readers = []


class Op:
    __slots__ = ("eng", "fn", "deps", "dma", "needs_inc", "count", "sem", "idx")

    def __init__(self, eng, fn, dma, idx):
        self.eng = eng
        self.fn = fn
        self.dma = dma
        self.deps = {}
        self.needs_inc = False
        self.count = None
        self.sem = None
        self.idx = idx


class Prog:
    ENGS = ("pe", "act", "dve", "pool", "sp")

    def __init__(self):
        self.ops = []
        self.n_dma = 0
        self.last_on_dsem = {}
        self.last_eng = {}
        self.bar = {}

    def barrier(self):
        last = dict(self.last_eng)
        self.bar = {e: [o for f, o in last.items() if f != e] for e in self.ENGS}

    def add(self, eng, fn, reads=(), writes=(), dma=False):
        op = Op(eng, fn, dma, len(self.ops))
        for r in reads:
            if r.lw is not None:
                op.deps[r.lw] = "RAW"
        for r in writes:
            if r.lw is not None and r.lw not in op.deps:
                op.deps[r.lw] = "WAW"
            for rd in r.readers:
                if rd not in op.deps:
                    op.deps[rd] = "WAR"
        op.deps.pop(op, None)
        if self.bar.get(eng):
            for o in self.bar[eng]:
                if o.dma:
                    continue
                op.deps[o] = "BAR"
            self.bar[eng] = None
        for r in reads:
            r.readers.append(op)
        for r in writes:
            r.lw = op
            r.readers = []
        if dma:
            j = self.n_dma % N_DMA_SEMS
            self.n_dma += 1
            op.sem = j
            prev = self.last_on_dsem.get(j)
            if prev is not None and prev not in op.deps:
                op.deps[prev] = "SEM"
            self.last_on_dsem[j] = op
        else:
            self.last_eng[eng] = op
        self.ops.append(op)
        return op

    def pe(self, fn, reads=(), writes=()):
        return self.add("pe", fn, reads, writes)

    def act(self, fn, reads=(), writes=()):
        return self.add("act", fn, reads, writes)

    def dve(self, fn, reads=(), writes=()):
        return self.add("dve", fn, reads, writes)

    def pool(self, fn, reads=(), writes=()):
        return self.add("pool", fn, reads, writes)

    def dma(self, q, fn, reads=(), writes=()):
        return self.add(q, fn, reads, writes, dma=True)

    def emit(self, nc, final_wait_ops=()):
        ops = self.ops
        need = {}
        for op in ops:
            lst = []
            best = {}
            for d, kind in op.deps.items():
                if d.dma:
                    lst.append(d)
                elif d.eng == op.eng and not op.dma:
                    if op.eng == "pe":
                        continue
                    if kind == "RAW" and (d.eng not in best or best[d.eng].idx < d.idx):
                        best[d.eng] = d
                elif d.eng not in best or best[d.eng].idx < d.idx:
                    best[d.eng] = d
            for d in best.values():
                d.needs_inc = True
                lst.append(d)
            need[op] = lst
        cnt = {e: 0 for e in self.ENGS}
        dcnt = {}
        for op in ops:
            if op.dma:
                dcnt[op.sem] = dcnt.get(op.sem, 0) + 16
                op.count = dcnt[op.sem]
            elif op.needs_inc:
                cnt[op.eng] += 1
                op.count = cnt[op.eng]
        per_eng = {e: [o for o in ops if o.eng == e] for e in self.ENGS}
        with contextlib.ExitStack() as st:
            esem = {e: st.enter_context(nc.semaphore("s_" + e)) for e in self.ENGS}
            dsem = [st.enter_context(nc.semaphore("d%d" % i)) for i in range(N_DMA_SEMS)]
            block = st.enter_context(nc.Block())

            def run(ename, eng):
                seen = {}
                for op in per_eng[ename]:
                    for d in need[op]:
                        if d.dma:
                            key = ("d", d.sem)
                            sem = dsem[d.sem]
                        else:
                            key = ("e", d.eng)
                            sem = esem[d.eng]
                        if seen.get(key, 0) >= d.count:
                            continue
                        seen[key] = d.count
                        eng.wait_ge(sem, d.count)
                    ins = op.fn(eng)
                    if op.dma:
                        ins.then_inc(dsem[op.sem], 16)
                    elif op.needs_inc:
                        ins.then_inc(esem[ename], 1)
                for d in final_wait_ops:
                    if d.eng == ename:
                        key = ("d", d.sem)
                        if seen.get(key, 0) < d.count:
                            seen[key] = d.count
                            eng.wait_ge(dsem[d.sem], d.count)

            @block.tensor
            def _(eng):
                run("pe", eng)

            @block.scalar
            def _(eng):
                run("act", eng)

            @block.vector
            def _(eng):
                run("dve", eng)

            @block.gpsimd
            def _(eng):
                run("pool", eng)

            @block.sync
            def _(eng):
                run("sp", eng)


class Arena:
    def __init__(self, t, lo, hi):
        self.t = t
        self.lo = lo
        self.hi = hi
        self.off = lo

    def mark(self):
        return self.off

    def reset(self, m=None):
        self.off = self.lo if m is None else m

    def alloc(self, cols, name="a"):
        cols_al = (cols + 1) // 2 * 2
        assert self.off + cols_al <= self.hi, (name, self.off, cols_al, self.hi)
        ap = self.t[:, self.off:self.off + cols]
        self.off += cols_al
        return ap, Reg(name)

    def alloc_f32(self, cols, name="a"):
        ap, r = self.alloc(2 * cols, name)
        return ap.bitcast(F32), r


def bview(ap2d, inner):
    a = ap2d.ap
    return bass.AP(ap2d.tensor, ap2d.offset, [list(a[0]), list(a[1]), [0, inner]])


def bview_mid(ap2d, g, n):
    a = ap2d.ap
    return bass.AP(ap2d.tensor, ap2d.offset, [list(a[0]), [n, g], [0, n], [1, n]])


def bview_in(ap2d, g, n):
    a = ap2d.ap
    return bass.AP(ap2d.tensor, ap2d.offset, [list(a[0]), [n, g], [1, n], [0, n]])


def _consts():
    half = 32
    inv = (10000.0 ** (-np.arange(0, 64, 2, dtype=np.float32) / np.float32(64))).astype(np.float32)
    ang = np.arange(S, dtype=np.float32)[:, None] * inv[None, :]
    ang = np.concatenate([ang, ang], axis=-1)
    cos = np.cos(ang).astype(np.float32).T
    sin = np.sin(ang).astype(np.float32).T
    sgn = np.concatenate([-np.ones(half), np.ones(half)]).astype(np.float32)[:, None]
    cosT = np.concatenate([cos, cos], 0)
    sinT = np.concatenate([sin * sgn, sin * sgn], 0)
    p = np.arange(128)[:, None]
    c = np.arange(2560)[None, :]
    dl = c - p - 384
    w = ((dl >= 0) & (dl <= 128)).astype(np.float32) + ((dl >= 0) & (dl <= 512) & (dl % 4 == 0)) \
        + ((dl >= 0) & (dl <= 2048) & (dl % 16 == 0))
    Tb = w.astype(np.float32)
    c2 = np.arange(640)[None, :]
    Tc = ((c2 - p - 384) >= 0).astype(np.float32)
    onehot = np.zeros((128, 8, 128), np.float32)
    for b in range(8):
        onehot[b, b, :] = 1.0
    ident = np.eye(128, dtype=np.float32)
    sw = (p // 64) * 64 + ((p % 64) + 32) % 64
    rperm = (np.arange(128)[:, None] == sw.T).astype(np.float32)
    rperm = rperm.reshape(128, 128)
    blockones = ((np.arange(128)[:, None] // 64) == (np.arange(128)[None, :] // 64)).astype(np.float32)
    ones = np.ones((128, 128), np.float32)
    gmask = np.zeros((128, NT, 4, 8), np.float32)
    for t in range(NT):
        qb = t // 2
        for n in range(8):
            gmask[:, t, :, n] = 0.0 if n < qb else (1e30 if n == qb else -1e30)
    bf = np.concatenate([cosT, sinT, Tb, Tc, onehot.reshape(128, -1), ident, rperm, blockones, ones], axis=1)
    return np.ascontiguousarray(bf, dtype=np.float32), np.ascontiguousarray(gmask.reshape(128, -1))


C_COS, C_SIN, C_TB, C_TC, C_OH, C_ID, C_RP, C_BO, C_ON = 0, 2048, 4096, 6656, 7296, 8320, 8448, 8576, 8704
C_TOT = 8832


def _pvec(inp, l):
    def hd(v, swap=False):
        d = np.arange(128) % 64
        if swap:
            d = (d + 32) % 64
        return v[d]
    cols = []
    for nm in ("q_norm_a", "k_norm_a", "q_norm_b", "k_norm_b"):
        cols += [hd(inp[nm][l]), hd(inp[nm][l], True)]
    cols += [hd(inp["q_norm_m"][l]), hd(inp["k_norm_m"][l])]
    for nm in ("conv_b", "conv_ln_g", "conv_ln_b"):
        cols += [inp[nm][l][0:128], inp[nm][l][128:256]]
    for ct in range(2):
        for j in range(31):
            cols.append(inp["conv_w"][l][j, ct * 128:(ct + 1) * 128])
    on = inp["out_norm"][l]
    for g0 in (0, 256, 768):
        for h in range(4):
            v = on[g0 + 64 * h:g0 + 64 * h + 64]
            cols.append(np.concatenate([v, v]))
    cols += [on[512:640], on[640:768]]
    for j in range(3):
        for ft in range(44):
            cols.append(inp["ffn_conv_w"][l][j, ft * 128:(ft + 1) * 128])
    for ft in range(44):
        cols.append(inp["ffn_conv_b"][l][ft * 128:(ft + 1) * 128])
    out = np.stack(cols, axis=1).astype(np.float32)
    assert out.shape == (128, NV)
    return out


def build(n_seq=2, n_layers=DEPTH, dbg=None):
    nc = bass.Bass("TRN2", target_bir_lowering=False)

    def din(name, shape):
        return nc.dram_tensor(name, list(shape), F32, kind="ExternalInput").ap()

    x_d = din("x", [2, S, D])
    mem_d = din("mem", [2, 256, D])
    w_in_d = din("w_in", [DEPTH, D, 2304])
    w_mem_d = din("w_mem_kv", [DEPTH, D, 512])
    w_co_d = din("w_conv_out", [DEPTH, 256, 256])
    w_out_d = din("w_out", [DEPTH, D, D])
    w_up_d = din("w_up", [DEPTH, D, 2 * DFF])
    w_dn_d = din("w_down", [DEPTH, DFF, D])
    nmix_d = din("norm_mix", [DEPTH, D])
    nffn_d = din("norm_ffn", [DEPTH, D])
    nmem_d = din("mem_norm", [DEPTH, D])
    pvec_d = din("pvec", [DEPTH, 128, NV])
    cst_d = din("cst", [128, C_TOT])
    gmask_d = din("gmask", [128, 512])
    out_d = nc.dram_tensor("out", [2, S, D], F32, kind="ExternalOutput").ap()
    scr_d = nc.dram_tensor("scr", [8, 512], F32, kind="Internal").ap()
    r_scr = [Reg("scr%d" % i) for i in range(8)]
    scr_i = [0]
    dbg_d = None
    if dbg is not None:
        dbg_d = nc.dram_tensor("dbg", [128, 16384], F32, kind="ExternalOutput").ap()

    P = Prog()

    def MM(out, lhsT, rhs, start, stop, reads, writes):
        P.pe(lambda e: e.matmul(out, lhsT, rhs, start=start, stop=stop), reads, writes)

    def ACT(out, in_, func, reads, writes, bias=None, scale=None, accum=None):
        kw = {}
        if bias is not None:
            kw["bias"] = bias
        if scale is not None:
            kw["scale"] = scale
        if accum is not None:
            kw["accum_out"] = accum
        P.act(lambda e: e.activation(out, in_, func, **kw), reads, writes)

    def ACOPY(out, in_, reads, writes):
        P.act(lambda e: e.copy(out, in_), reads, writes)

    def ENG(eng):
        return {"dve": P.dve, "pool": P.pool}[eng]

    def TT(eng, out, in0, in1, op, reads, writes):
        ENG(eng)(lambda e: e.tensor_tensor(out, in0, in1, op), reads, writes)

    def TS(eng, out, in0, s1, s2, op0, op1, reads, writes):
        if op1 is None:
            ENG(eng)(lambda e: e.tensor_scalar(out, in0, s1, None, op0), reads, writes)
        else:
            ENG(eng)(lambda e: e.tensor_scalar(out, in0, s1, s2, op0, op1), reads, writes)

    def STT(eng, out, in0, sc, in1, op0, op1, reads, writes):
        ENG(eng)(lambda e: e.scalar_tensor_tensor(out, in0, sc, in1, op0, op1), reads, writes)

    def RECIP(out, in_, reads, writes):
        P.dve(lambda e: e.reciprocal(out, in_), reads, writes)

    def RED(out, in_, reads, writes):
        P.dve(lambda e: e.tensor_reduce(out, in_, AX.X, ALU.add), reads, writes)

    def CP(eng, out, in_, reads, writes):
        ENG(eng)(lambda e: e.tensor_copy(out, in_), reads, writes)

    def MEMSET(eng, ap, val, writes):
        ENG(eng)(lambda e: e.memset(ap, val), (), writes)

    def DMA(q, out, in_, reads=(), writes=()):
        return P.dma(q, lambda e: e.dma_start(out=out, in_=in_), reads, writes)

    st = contextlib.ExitStack()
    with st:
        def sb(name, shape, dt):
            return st.enter_context(nc.sbuf_tensor(name, shape, dt))

        def ps(name, shape, dt):
            return st.enter_context(nc.psum_tensor(name, shape, dt))

        X = sb("X", [128, NT, D], F32)
        rX = [Reg("x%d" % t) for t in range(NT)]
        pv = sb("pv", [128, DEPTH, NV], F32)
        r_pv = Reg("pv")
        cb = sb("cb", [128, 1536], BF16)
        r_cb = Reg("cb")
        onesf = sb("onesf", [128, 64], F32)
        r_onesf = Reg("onesf")
        gm = sb("gm", [128, 512], F32)
        r_gm = Reg("gm")
        stat = sb("stat", [128, 32], F32)
        r_stat = Reg("stat")
        NB = 57600
        NF = 4700
        arB_t = sb("arB", [128, NB], BF16)
        arF_t = sb("arF", [128, NF], F32)
        AB = Arena(arB_t, 0, NB)
        AFa = Arena(arF_t, 0, NF)
        pb = [ps("pb%d" % i, [128, 512], F32) for i in range(7)]
        r_pb = [Reg("pb%d" % i) for i in range(7)]
        ptr = ps("ptr", [128, 1024], BF16)
        r_ptr = Reg("ptr")

        if dbg is not None:
            tq = sb("dbgbuf", [128, 512], F32)
            r_tq = Reg("dbgbuf")
        ident = cb[:, 0:128]
        rperm = cb[:, 128:256]
        bones = cb[:, 256:384]
        ones = cb[:, 384:512]
        onehot = cb[:, 512:1536]

        DMA("sp", pv[:], pvec_d.rearrange("l p n -> p l n"), writes=[r_pv])
        DMA("pool", cb[:, 0:512], cst_d[:, C_ID:C_ID + 512], writes=[r_cb])
        DMA("pool", cb[:, 512:1536], cst_d[:, C_OH:C_OH + 1024], writes=[r_cb])
        DMA("sp", gm[:], gmask_d, writes=[r_gm])
        MEMSET("dve", onesf[:], 1.0, [r_onesf])
        idf = sb("idf", [128, 128], F32)
        r_idf = Reg("idf")
        DMA("sp", idf[:], cst_d[:, C_ID:C_ID + 128], writes=[r_idf])
        rsT2 = [(sb("rsT%d" % i, [128, 8], F32), Reg("rsT%d" % i)) for i in range(2)]
        hsel = cb[:, 256:384:64]
        epsb = sb("epsb", [128, 1], F32)
        r_eps = Reg("eps")
        MEMSET("dve", epsb[:], EPS, [r_eps])

        def RSQ(dst, src, scale, reads, r_dst, extra_writes=(), ln=False):
            if not ln:
                ACT(dst, src, AF.Sqrt, list(reads) + [r_eps], [r_dst] + list(extra_writes),
                    bias=epsb[0:dst.shape[0], :] if dst.shape[0] != 128 else epsb[:], scale=scale)
                RECIP(dst, dst, [r_dst], [r_dst])
                return
            ACT(dst, src, AF.Ln, list(reads) + [r_eps], [r_dst] + list(extra_writes), bias=epsb[0:dst.shape[0], :] if dst.shape[0] != 128 else epsb[:], scale=scale)
            ACT(dst, dst, AF.Exp, [r_dst], [r_dst], scale=-0.5)

        def rms_to_hT(src_tiles, r_src, gain_row, tmp, hT, r_hT_of, tok_of):
            gB, r_g = tmp["gain"]
            junk, r_junk = tmp["junk"]
            ht = tmp["ht"]
            n_tiles = len(src_tiles)
            DMA("sp", gB, gain_row.partition_broadcast(128), writes=[r_g])
            for t in range(n_tiles):
                ACT(junk, src_tiles[t], AF.Square, [r_src[t]], [r_junk, r_stat], accum=stat[:, t:t + 1])
            sl = stat[:, 0:n_tiles]
            RSQ(sl, sl, 1.0 / D, [r_stat], r_stat)
            for t in range(n_tiles):
                hb, r_hb = ht[t % 2]
                STT("dve", hb, src_tiles[t], stat[:, t:t + 1], gB, ALU.mult, ALU.mult, [r_src[t], r_stat, r_g], [r_hb])
                tb, r_tb = (ptr[:, :], r_ptr) if t % 2 == 0 else (pb[6][:, :].bitcast(BF16), r_pb[6])
                for c in range(8):
                    P.pe((lambda o_, i_: (lambda e: e.transpose(o_, i_, ident)))(tb[:, c * 128:(c + 1) * 128], hb[:, c * 128:(c + 1) * 128]),
                         [r_hb, r_cb], [r_tb])
                o = tok_of(t)
                ACOPY(hT[:, :, o:o + 128], tb.rearrange("p (c t) -> p c t", c=8), [r_tb], [r_hT_of(t)])

        out_ops = []
        dbg_ops = []

        for s in range(n_seq):
            for t in range(NT):
                DMA("sp", X[:, t, :], x_d[s, t * 128:(t + 1) * 128, :], writes=[rX[t]])
            for l in range(n_layers):
                def pcol(c, l=l):
                    return pv[:, l, c:c + 1]
                P.barrier()
                AB.reset()
                AFa.reset()
                qk_ap, _ = AB.alloc(5 * 2 * S, "qk")
                qk = qk_ap.rearrange("p (k c t) -> p k c t", k=5, c=2)
                r_qk = [[[Reg("qk") for j in range(4)] for c in range(2)] for k in range(5)]
                Vp_ap, _ = AB.alloc(NT * 8 * 65, "Vp")
                Vp = Vp_ap.rearrange("p (t h d) -> p t h d", t=NT, h=8)
                r_Vp = [Reg("Vp%d" % t) for t in range(NT)]
                Vm_ap, r_Vm = AB.alloc(2 * 4 * 65, "Vm")
                Vm = Vm_ap.rearrange("p (t h d) -> p t h d", t=2, h=4)
                kmT_ap, r_kmT = AB.alloc(2 * 256, "kmT")
                kmT = kmT_ap.rearrange("p (c t) -> p c t", c=2)
                uT_off = AB.mark()
                uT_ap, _ = AB.alloc(2 * 2080, "uT")
                uT = uT_ap.rearrange("p (c t) -> p c t", c=2)
                r_uT = [[Reg("uT") for j in range(4)] for c in range(2)]
                base_m = AB.mark()
                base_f = AFa.mark()

                MEMSET("pool", Vp[:, :, :, 64:65], 1.0, r_Vp)
                MEMSET("pool", Vm[:, :, :, 64:65], 1.0, [r_Vm])
                MEMSET("pool", uT[:, :, 0:32], 0.0, [r_uT[0][0], r_uT[1][0]])

                memt_ap, _ = AFa.alloc(2 * D, "memt")
                memt = memt_ap.rearrange("p (j d) -> p j d", j=2)
                r_memt = [Reg("memt0"), Reg("memt1")]
                for j in range(2):
                    DMA("sp", memt[:, j, :], mem_d[s, j * 128:(j + 1) * 128, :], writes=[r_memt[j]])
                mhT_ap, r_mhT = AB.alloc(8 * 256, "mhT")
                mhT = mhT_ap.rearrange("p (c t) -> p c t", c=8)
                wm_ap, r_wm = AB.alloc(8 * 512, "wm")
                wm = wm_ap.rearrange("p (c n) -> p c n", c=8)
                DMA("pool", wm, w_mem_d[l].rearrange("(c p) n -> p c n", p=128), writes=[r_wm])
                tmp = dict(gain=AFa.alloc(D, "gain"), junk=AB.alloc(D, "junk"), ht=[AB.alloc(D, "ht") for _ in range(2)])
                rms_to_hT([memt[:, t, :] for t in range(2)], r_memt, nmem_d[l:l + 1, :], tmp, mhT, lambda t: r_mhT, lambda t: t * 128)
                sqm, r_sqm = AB.alloc(256, "sqm")
                rsm, r_rsm = AFa.alloc(256, "rsm")
                for ct in range(2):
                    A_, rA = pb[ct], r_pb[ct]
                    for c in range(8):
                        MM(A_[:, 0:256], wm[:, c, ct * 128:(ct + 1) * 128], mhT[:, c, :], c == 0, c == 7, [r_wm, r_mhT], [rA])
                    ACT(sqm, A_[:, 0:256], AF.Square, [rA], [r_sqm])
                    B_, rB = pb[2 + ct], r_pb[2 + ct]
                    MM(B_[:, 0:256], bones, sqm, True, True, [r_sqm, r_cb], [rB])
                    RSQ(rsm, B_[:, 0:256], 1.0 / 64, [rB], r_rsm)
                    STT("dve", kmT[:, ct, :], A_[:, 0:256], pcol(9), rsm, ALU.mult, ALU.mult, [rA, r_rsm, r_pv], [r_kmT])
                for j in range(2):
                    A_, rA = pb[4 + j], r_pb[4 + j]
                    for c in range(8):
                        MM(A_[:, 0:256], mhT[:, c, j * 128:(j + 1) * 128], wm[:, c, 256:512], c == 0, c == 7, [r_wm, r_mhT], [rA])
                    ACOPY(Vm[:, j, :, 0:64], A_[:, 0:256].rearrange("p (h d) -> p h d", h=4), [rA], [r_Vm])
                P.barrier()
                AB.reset(base_m)
                AFa.reset(base_f)

                cosb, r_cos = AB.alloc(S, "cos")
                sinb, r_sin = AB.alloc(S, "sin")
                DMA("pool", cosb, cst_d[:, C_COS:C_COS + S], writes=[r_cos])
                DMA("pool", sinb, cst_d[:, C_SIN:C_SIN + S], writes=[r_sin])
                hT_ap, _ = AB.alloc(8 * 1024, "hT")
                hT = hT_ap.rearrange("p (c t) -> p c t", c=8)
                r_hT = [Reg("hT0"), Reg("hT1")]
                wst = []
                for i in range(2):
                    a, r = AB.alloc(8 * 256, "wst")
                    wst.append((a.rearrange("p (c n) -> p c n", c=8), r))
                sqb = [AB.alloc(512, "sq") for _ in range(2)]
                qbb = [AB.alloc(512, "qb") for _ in range(2)]
                sigb = {(ct, jc): AB.alloc(512, "sig") for ct in range(2) for jc in range(2)}
                rsb = [AFa.alloc(512, "rs") for _ in range(2)]
                t1, r_t1 = AFa.alloc(512, "t1")
                t2, r_t2 = AFa.alloc(512, "t2")
                tmp = dict(gain=AFa.alloc(D, "gain"), junk=AB.alloc(D, "junk"), ht=[AB.alloc(D, "ht") for _ in range(2)])
                w_l = w_in_d[l].rearrange("(c p) n -> p c n", p=128)
                strips = [(0, "qk", 0), (256, "qk", 1), (768, "qk", 2), (1024, "qk", 3), (2048, "qm", 4),
                          (1792, "cg", None), (1536, "cv", None)]
                si = 0
                all_strips = []
                for hf in range(2):
                    all_strips.append((hf, 512, "v", 0))
                    all_strips.append((hf, 1280, "v", 1))
                    for (col0, kind, qi) in strips:
                        all_strips.append((hf, col0, kind, qi))

                def load_strip(n):
                    if n >= len(all_strips):
                        return
                    wS_, r_wS_ = wst[n % 2]
                    c0 = all_strips[n][1]
                    DMA("pool", wS_, w_l[:, :, c0:c0 + 256], writes=[r_wS_])
                load_strip(0)
                gi = 0
                for hf in range(2):
                    rms_to_hT([X[:, hf * 8 + t, :] for t in range(8)], rX[hf * 8:hf * 8 + 8], nmix_d[l:l + 1, :], tmp, hT,
                              lambda t: r_hT[t // 4], lambda t: t * 128)
                    tiles = []
                    for sn in range(hf * 9, hf * 9 + 9):
                        _, col0, kind, qi = all_strips[sn]
                        wS, r_wS = wst[sn % 2]
                        if kind == "v":
                            load_strip(sn + 1)
                            for t in range(8):
                                A_, rA = pb[5 + (t % 2)], r_pb[5 + (t % 2)]
                                for c in range(8):
                                    MM(A_[:, 0:256], hT[:, c, t * 128:(t + 1) * 128], wS[:, c, :], c == 0, c == 7, [r_wS, r_hT[t // 4]], [rA])
                                tt = hf * 8 + t
                                ACOPY(Vp[:, tt, qi * 4:qi * 4 + 4, 0:64], A_[:, 0:256].rearrange("p (h d) -> p h d", h=4), [rA], [r_Vp[tt]])
                            continue
                        for ct in range(2):
                            for jc in range(2):
                                tiles.append(dict(sn=sn, first=(ct == 0 and jc == 0), wS=wS, r_wS=r_wS, kind=kind, qi=qi, ct=ct, jc=jc, gi=gi))
                                gi += 1

                    def stA(u):
                        wS, r_wS, kind, ct, jc, k = u["wS"], u["r_wS"], u["kind"], u["ct"], u["jc"], u["gi"]
                        if u["first"]:
                            load_strip(u["sn"] + 1)
                        tk = jc * 512
                        g0 = (hf * 2 + jc) * 512
                        A_, rA = pb[k % 3], r_pb[k % 3]
                        for c in range(8):
                            MM(A_[:, :], wS[:, c, ct * 128:(ct + 1) * 128], hT[:, c, tk:tk + 512], c == 0, c == 7, [r_wS, r_hT[jc]], [rA])
                        if kind in ("qk", "qm"):
                            sq, r_sq = sqb[k % 2]
                            ACT(sq, A_[:, :], AF.Square, [rA], [r_sq])
                            if kind == "qk":
                                qb_, r_qb = qbb[k % 2]
                                ACOPY(qb_, A_[:, :], [rA], [r_qb])
                        elif kind == "cg":
                            sg, r_sg = sigb[(ct, jc)]
                            ACT(sg, A_[:, :], AF.Sigmoid, [rA], [r_sg])
                        else:
                            sg, r_sg = sigb[(ct, jc)]
                            TT("dve", uT[:, ct, 32 + g0:32 + g0 + 512], A_[:, :], sg, ALU.mult, [rA, r_sg], [r_uT[ct][hf * 2 + jc]])

                    def stB(u):
                        kind, k = u["kind"], u["gi"]
                        if kind not in ("qk", "qm"):
                            return
                        sq, r_sq = sqb[k % 2]
                        rs, r_rs = rsb[k % 2]
                        B_, rB = pb[3 + (k % 2)], r_pb[3 + (k % 2)]
                        if QK_COMPACT:
                            rsT, r_rsT = rsT2[k % 2]
                            for sub in range(4):
                                MM(B_[:, sub * 2:sub * 2 + 2], sq[:, sub * 128:(sub + 1) * 128], hsel, True, True, [r_sq, r_cb], [rB])
                        else:
                            MM(B_[:, :], bones, sq, True, True, [r_sq, r_cb], [rB])
                        if kind == "qk":
                            qb_, r_qb = qbb[k % 2]
                            C_, rC = pb[5 + (k % 2)], r_pb[5 + (k % 2)]
                            MM(C_[:, :], rperm, qb_, True, True, [r_qb, r_cb], [rC])
                        if QK_COMPACT:
                            ACT(rsT[:], B_[:, 0:8], AF.Sqrt, [rB, r_eps], [r_rsT], bias=epsb[:], scale=1.0 / 64)
                        elif QK_LN:
                            ACT(rs, B_[:, :], AF.Ln, [rB, r_eps], [r_rs], bias=epsb[:], scale=1.0 / 64)
                            ACT(rs, rs, AF.Exp, [r_rs], [r_rs], scale=-0.5)
                        else:
                            ACT(rs, B_[:, :], AF.Sqrt, [rB, r_eps], [r_rs], bias=epsb[:], scale=1.0 / 64)

                    def stC(u):
                        kind, qi, ct, jc, k = u["kind"], u["qi"], u["ct"], u["jc"], u["gi"]
                        if kind not in ("qk", "qm"):
                            return
                        j = hf * 2 + jc
                        g0 = j * 512
                        A_, rA = pb[k % 3], r_pb[k % 3]
                        rs, r_rs = rsb[k % 2]
                        if QK_COMPACT:
                            rsT, r_rsT = rsT2[k % 2]
                            B_, rB = pb[3 + (k % 2)], r_pb[3 + (k % 2)]
                            RECIP(rsT[:], rsT[:], [r_rsT], [r_rsT])
                            CP("dve", rs.rearrange("p (a d) -> p a d", d=64), bview(rsT[:, 0:8], 64), [r_rsT], [r_rs])
                            for sub in range(4):
                                MM(B_[:, sub * 128:(sub + 1) * 128], rs[:, sub * 128:(sub + 1) * 128], idf[:], True, True, [r_rs, r_idf], [rB])
                            rs, r_rs = B_[:, :], rB
                        elif not QK_LN:
                            RECIP(rs, rs, [r_rs], [r_rs])
                        dst = qk[:, qi, ct, g0:g0 + 512]
                        r_dst = r_qk[qi][ct][j]
                        if kind == "qm":
                            if QK_COMPACT:
                                ACT(t1, A_[:, :], AF.Copy, [rA, r_pv], [r_t1], scale=pcol(8))
                                TT("dve", dst, t1, rs, ALU.mult, [r_t1, r_rs], [r_dst])
                            else:
                                STT("dve", dst, A_[:, :], pcol(8), rs, ALU.mult, ALU.mult, [rA, r_rs, r_pv], [r_dst])
                        else:
                            C_, rC = pb[5 + (k % 2)], r_pb[5 + (k % 2)]
                            gc = 2 * qi
                            STT("dve", t1, A_[:, :], pcol(gc), cosb[:, g0:g0 + 512], ALU.mult, ALU.mult, [rA, r_cos, r_pv], [r_t1])
                            STT("dve", t2, C_[:, :], pcol(gc + 1), sinb[:, g0:g0 + 512], ALU.mult, ALU.mult, [rC, r_sin, r_pv], [r_t2])
                            TT("dve", t1, t1, t2, ALU.add, [r_t1, r_t2], [r_t1])
                            TT("dve", dst, t1, rs, ALU.mult, [r_t1, r_rs], [r_dst])

                    n_u = len(tiles)
                    for i in range(n_u + 2):
                        if i < n_u:
                            stA(tiles[i])
                        if 0 <= i - 1 < n_u:
                            stB(tiles[i - 1])
                        if 0 <= i - 2 < n_u:
                            stC(tiles[i - 2])
                if dbg == "qk":
                    for k in range(4):
                        for c in range(2):
                            CP("dve", tq[:], qk[:, k, c, 0:512], r_qk[k][c], [r_tq])
                            dbg_ops.append(DMA("sp", dbg_d[:, (k * 2 + c) * 512:(k * 2 + c + 1) * 512], tq[:], reads=[r_tq]))
                P.barrier()
                AB.reset(base_m)
                AFa.reset(base_f)

                ocT_ap, _ = AB.alloc(2 * S, "ocT")
                ocT = ocT_ap.rearrange("p (c t) -> p c t", c=2)
                r_ocT = [[Reg("ocT") for j in range(4)] for c in range(2)]
                keep_m = AB.mark()
                dg_ap, r_dg = AB.alloc(2 * 31 * 128, "dg")
                dg = dg_ap.rearrange("p (c j n) -> p c j n", c=2, j=31)
                wco_ap, r_wco = AB.alloc(2 * 256, "wco")
                wco = wco_ap.rearrange("p (c n) -> p c n", c=2)
                DMA("pool", wco, w_co_d[l].rearrange("(c p) n -> p c n", p=128), writes=[r_wco])
                for ct in range(2):
                    for j in range(31):
                        TS("dve", dg[:, ct, j, :], ident, pcol(16 + ct * 31 + j), None, ALU.mult, None, [r_cb, r_pv], [r_dg])
                yf = [AFa.alloc(512, "yf") for _ in range(2)]
                ybf = [AB.alloc(512, "ybf") for _ in range(2)]
                ysq = [AB.alloc(512, "ysq") for _ in range(2)]
                zs = [AB.alloc(512, "zs") for _ in range(2)]
                mean, r_mean = AFa.alloc(512, "mean")
                msq, r_msq = AFa.alloc(512, "msq")
                rstd, r_rstd = AFa.alloc(512, "rstd")
                dd, r_dd = AFa.alloc(512, "dd")
                for j in range(4):
                    g0 = j * 512
                    for ct in range(2):
                        A_, rA = pb[ct], r_pb[ct]
                        rds = [r_uT[ct][j], r_dg] + ([r_uT[ct][j - 1]] if j > 0 else [])
                        for tp in range(31):
                            MM(A_[:, :], dg[:, ct, tp, :], uT[:, ct, 2 + tp + g0:2 + tp + g0 + 512], tp == 0, tp == 30, rds, [rA])
                        ACT(yf[ct][0], A_[:, :], AF.Identity, [rA, r_pv], [yf[ct][1]], bias=pcol(10 + ct))
                        ACT(ysq[ct][0], A_[:, :], AF.Square, [rA, r_pv], [ysq[ct][1]], bias=pcol(10 + ct))
                        CP("dve", ybf[ct][0], yf[ct][0], [yf[ct][1]], [ybf[ct][1]])
                    for ct in range(2):
                        MM(pb[2][:, :], ones, ybf[ct][0], ct == 0, ct == 1, [ybf[ct][1], r_cb], [r_pb[2]])
                    for ct in range(2):
                        MM(pb[3][:, :], ones, ysq[ct][0], ct == 0, ct == 1, [ysq[ct][1], r_cb], [r_pb[3]])
                    TS("dve", mean, pb[2][:, :], 1.0 / 256, None, ALU.mult, None, [r_pb[2]], [r_mean])
                    TT("dve", msq, mean, mean, ALU.mult, [r_mean], [r_msq])
                    STT("dve", rstd, pb[3][:, :], 1.0 / 256, msq, ALU.mult, ALU.subtract, [r_pb[3], r_msq], [r_rstd])
                    RSQ(rstd, rstd, 1.0, [r_rstd], r_rstd)
                    for ct in range(2):
                        TT("dve", dd, yf[ct][0], mean, ALU.subtract, [yf[ct][1], r_mean], [r_dd])
                        TT("dve", dd, dd, rstd, ALU.mult, [r_dd, r_rstd], [r_dd])
                        TS("dve", dd, dd, pcol(12 + ct), pcol(14 + ct), ALU.mult, ALU.add, [r_dd, r_pv], [r_dd])
                        ACT(zs[ct][0], dd, AF.Silu, [r_dd], [zs[ct][1]])
                    for c2 in range(2):
                        O_, rO = pb[4 + c2], r_pb[4 + c2]
                        for ct in range(2):
                            MM(O_[:, :], wco[:, ct, c2 * 128:(c2 + 1) * 128], zs[ct][0], ct == 0, ct == 1, [zs[ct][1], r_wco], [rO])
                        ACOPY(ocT[:, c2, g0:g0 + 512], O_[:, :], [rO], [r_ocT[c2][j]])
                if dbg == "oc":
                    for c in range(2):
                        for j in range(4):
                            CP("dve", tq[:], ocT[:, c, j * 512:(j + 1) * 512], [r_ocT[c][j]], [r_tq])
                            dbg_ops.append(DMA("sp", dbg_d[:, (c * 4 + j) * 512:(c * 4 + j + 1) * 512], tq[:], reads=[r_tq]))
                P.barrier()
                AB.reset(keep_m)
                AFa.reset(base_f)
                AU = Arena(arB_t, uT_off, uT_off + 2 * 2080)

                Tb, r_Tb = AB.alloc(2560, "Tb")
                Tc, r_Tc = AB.alloc(640, "Tc")
                DMA("pool", Tb, cst_d[:, C_TB:C_TB + 2560], writes=[r_Tb])
                DMA("pool", Tc, cst_d[:, C_TC:C_TC + 640], writes=[r_Tc])
                negm_ap, r_negm = AB.alloc(NT * 4 * 8, "negm")
                negm = negm_ap.rearrange("p (t h n) -> p t h n", t=NT, h=4)
                ksum, r_ksum = AFa.alloc(16, "ksum")
                kmean_ap, r_kmean = AB.alloc(16, "kmean")
                kmean = kmean_ap.rearrange("p (c n) -> p c n", c=2)
                for c in range(2):
                    RED(ksum[:, c * 8:(c + 1) * 8], qk[:, 1, c, :].rearrange("p (n k) -> p n k", n=8), r_qk[1][c], [r_ksum])
                TS("dve", kmean_ap, ksum, 1.0 / 256, None, ALU.mult, None, [r_ksum], [r_kmean])
                G_, rG = pb[0], r_pb[0]
                for t in range(NT):
                    for h in range(4):
                        r0 = (h % 2) * 64
                        gcol = (t * 4 + h) * 8
                        MM(G_[:, gcol:gcol + 8], qk[r0:r0 + 64, 0, h // 2, t * 128:(t + 1) * 128], kmean[r0:r0 + 64, h // 2, :], True, True,
                           [r_qk[0][h // 2][t // 4], r_kmean], [rG])
                gate, r_gate = AFa.alloc(512, "gate")
                TT("dve", gate, G_[:, :], gm[:], ALU.add, [rG, r_gm], [r_gate])
                cmpb, r_cmp = AFa.alloc(2048, "cmp")
                rank, r_rank = AFa.alloc(512, "rank")
                for hh in range(2):
                    gsl = gate[:, hh * 256:(hh + 1) * 256]
                    cmp4 = cmpb.rearrange("p (g n m) -> p g n m", g=32, n=8)
                    TT("dve", cmp4, bview_mid(gsl, 32, 8), bview_in(gsl, 32, 8), ALU.is_gt, [r_gate], [r_cmp])
                    RED(rank[:, hh * 256:(hh + 1) * 256], cmpb.rearrange("p (g m) -> p g m", m=8), [r_cmp], [r_rank])
                TS("dve", negm_ap, rank, 4.0, NEG, ALU.is_ge, ALU.mult, [r_rank], [r_negm])
                if dbg == "gate":
                    dbg_ops.append(DMA("sp", dbg_d[:, 0:512], gate, reads=[r_gate]))
                    dbg_ops.append(DMA("sp", dbg_d[:, 512:1024], rank, reads=[r_rank]))

                AFa.reset(base_f)
                mixS_ap, _ = AB.alloc(8 * 512, "mixS")
                mixS = mixS_ap.rearrange("p (k t) -> p k t", k=8)
                r_mix = [Reg("mix%d" % k) for k in range(8)]
                mixO_ap, _ = AB.alloc(6 * 512, "mixO")
                mixO = mixO_ap.rearrange("p (k t) -> p k t", k=6)
                r_mixO = [Reg("mixO%d" % k) for k in range(6)]
                PT = [AU.alloc(512, "PT") for _ in range(3)] + [AB.alloc(512, "PT") for _ in range(7)]
                negT = [AU.alloc(512, "negT") for _ in range(2)]
                sqh = [AU.alloc(512, "sqh") for _ in range(3)] + [AB.alloc(512, "sqh")]
                wo_ap, _ = AB.alloc(8 * 512, "wo")
                wo = wo_ap.rearrange("p (k n) -> p k n", k=8)
                r_wo = [Reg("wo%d" % k) for k in range(8)]
                onh = [AFa.alloc(512, "onh") for _ in range(4)]
                rden2 = [AFa.alloc(512, "rden") for _ in range(2)]
                bcs2 = [AFa.alloc(512, "bcs") for _ in range(2)]
                rsg, r_rsg = AFa.alloc(512, "rsg")
                rows = [0, 128, 256, 384, 768, 896, 512, 640]
                epi = [0]
                pti = 0
                sti = 0
                pending = []

                pending_head = []

                def flush_head():
                    fs = list(pending_head)
                    del pending_head[:]
                    for f_ in fs:
                        f_()

                def flush_pending():
                    flush_head()
                    fs = list(pending)
                    del pending[:]
                    for f_ in fs:
                        f_()
                for qc in range(4):
                    q0 = qc * 512
                    for grp in range(3):
                        stream = []
                        if grp == 2:
                            tiles = [(kt, q0, 512) for kt in range(2)]
                        elif grp == 0:
                            tiles = []
                            for kt in range(4 * qc + 4):
                                ql = max(q0, 256 * (kt // 2))
                                tiles.append((kt, ql, q0 + 512 - ql))
                        else:
                            tiles = []
                            for kt in range(4 * qc + 4):
                                ql = max(q0, 128 * kt)
                                tiles.append((kt, ql, q0 + 512 - ql))
                        for p_ in range(2):
                            for ti, (kt, ql, N) in enumerate(tiles):
                                stream.append(dict(p=p_, ti=ti, nt=len(tiles), kt=kt, ql=ql, N=N))
                        SBK = [(pb[0][:, :], r_pb[0]), (pb[1][:, :], r_pb[1]), (pb[4][:, :], r_pb[4]), (pb[5][:, :], r_pb[5]),
                               (pb[6][:, :], r_pb[6]), (ptr[:, :].bitcast(F32), r_ptr)]

                        def stage1(it):
                            nonlocal sti, pti
                            p_, ti, kt, ql, N = it["p"], it["ti"], it["kt"], it["ql"], it["N"]
                            co = ql - q0
                            it["pt"] = []
                            sbk = []
                            for hh in range(2):
                                h = 2 * p_ + hh
                                nT, r_nT = negT[hh]
                                if grp == 0 and ti == 0:
                                    for t4 in range(4):
                                        t = qc * 4 + t4
                                        P.pe((lambda o_, i_: (lambda e: e.transpose(o_, i_, ident)))(ptr[0:8, t4 * 128:(t4 + 1) * 128], negm[:, t, h, :]),
                                             [r_negm, r_cb], [r_ptr])
                                    ACOPY(nT[0:8, :], ptr[0:8, 0:512], [r_ptr], [r_nT])
                                sbk.append(SBK[(sti % 3) * 2 + hh])
                                it["pt"].append(PT[pti % 10])
                                pti += 1
                            sti += 1
                            th = p_
                            for hh in range(2):
                                r0 = hh * 64
                                S_, rS = sbk[hh]
                                if grp == 2:
                                    MM(S_[:, 0:512], kmT[r0:r0 + 64, th, kt * 128:(kt + 1) * 128], qk[r0:r0 + 64, 4, th, q0:q0 + 512], True, True,
                                       [r_kmT, r_qk[4][th][qc]], [rS])
                                else:
                                    qi, ki = (0, 1) if grp == 0 else (2, 3)
                                    MM(S_[:, 0:N], qk[r0:r0 + 64, ki, th, kt * 128:(kt + 1) * 128], qk[r0:r0 + 64, qi, th, ql:ql + N], True, grp == 1,
                                       [r_qk[ki][th][kt // 4], r_qk[qi][th][qc]], [rS])
                            if grp == 0:
                                kb = kt // 2
                                for hh in range(2):
                                    S_, rS = sbk[hh]
                                    nT, r_nT = negT[hh]
                                    MM(S_[:, 0:N], onehot[0:8, kb * 128:(kb + 1) * 128], nT[0:8, co:co + N], False, True, [r_cb, r_nT], [rS])
                            for hh in range(2):
                                S_, rS = sbk[hh]
                                pt, r_pt = it["pt"][hh]
                                ACT(pt[:, 0:N], S_[:, 0:N], AF.Exp, [rS], [r_pt], scale=0.125)
                            for hh in range(2):
                                pt, r_pt = it["pt"][hh]
                                if grp == 0 and (kt // 2) >= 2 * qc:
                                    off = ql - 128 * kt + 384
                                    TT("dve", pt[:, 0:256], pt[:, 0:256], Tc[:, off:off + 256], ALU.mult, [r_pt, r_Tc], [r_pt])
                                if grp == 1:
                                    off = ql - 128 * kt + 384
                                    TT("dve", pt[:, 0:N], pt[:, 0:N], Tb[:, off:off + N], ALU.mult, [r_pt, r_Tb], [r_pt])

                        def stage2(it):
                            p_, ti, nt, kt, ql, N = it["p"], it["ti"], it["nt"], it["kt"], it["ql"], it["N"]
                            co = ql - q0
                            for hh in range(2):
                                h = 2 * p_ + hh
                                pt, r_pt = it["pt"][hh]
                                O_, rO = pb[2 + hh], r_pb[2 + hh]
                                if grp == 2:
                                    lhs, rl = Vm[:, kt, h, :], r_Vm
                                else:
                                    lhs, rl = Vp[:, kt, grp * 4 + h, :], r_Vp[kt]
                                MM(O_[0:65, co:co + N], lhs, pt[:, 0:N], ti == 0, ti == nt - 1, [rl, r_pt], [rO])
                            if pending_head and ti == min(3, nt - 1):
                                flush_head()
                            if ti == nt - 1:
                                if p_ == 0:
                                    flush_pending()
                                for hh in range(2):
                                    h = 2 * p_ + hh
                                    O_, rO = pb[2 + hh], r_pb[2 + hh]
                                    on, r_on = onh[h]
                                    sq, r_sq = sqh[h]
                                    rden, r_rden = rden2[hh]
                                    bcs, r_bcs = bcs2[hh]
                                    ACOPY(on[0:65, :], O_[0:65, :], [rO], [r_on])
                                    RECIP(rden[64:65, :], on[64:65, :], [r_on], [r_rden])
                                    si_ = scr_i[0] % 8
                                    scr_i[0] += 1
                                    DMA("sp", scr_d[si_:si_ + 1, :], rden[64:65, :], reads=[r_rden], writes=[r_scr[si_]])
                                    DMA("sp", bcs[0:64, :], scr_d[si_:si_ + 1, :].partition_broadcast(64), reads=[r_scr[si_]], writes=[r_bcs])

                                    def part1b(on=on, r_on=r_on, sq=sq, r_sq=r_sq, bcs=bcs, r_bcs=r_bcs):
                                        TT("dve", on[0:64, :], on[0:64, :], bcs[0:64, :], ALU.mult, [r_on, r_bcs], [r_on])
                                        ACT(sq[0:64, :], on[0:64, :], AF.Square, [r_on], [r_sq])
                                    pending_head.append(part1b)

                        n_it = len(stream)
                        for i_ in range(n_it + 2):
                            if i_ < n_it:
                                stage1(stream[i_])
                            if 0 <= i_ - 2 < n_it:
                                stage2(stream[i_ - 2])
                        def part2(grp=grp, l=l):
                            for h in range(4):
                                MM(pb[5][0:64, :], ones[0:64, 0:64], sqh[h][0][0:64, :], h == 0, h == 3, [sqh[h][1], r_cb], [r_pb[5]])
                            RSQ(rsg[0:64, :], pb[5][0:64, :], 1.0 / 256, [r_pb[5]], r_rsg)
                            for h in range(4):
                                k = grp * 4 + h
                                c = grp * 2 + h // 2
                                if h % 2 == 0:
                                    STT("dve", mixS[0:64, c, :], onh[h][0][0:64, :], pv[0:64, l, 78 + k:79 + k], rsg[0:64, :], ALU.mult, ALU.mult,
                                        [onh[h][1], r_rsg, r_pv], [r_mix[c]])
                                else:
                                    o_i = grp * 2 + h // 2
                                    STT("dve", mixO[0:64, o_i, :], onh[h][0][0:64, :], pv[0:64, l, 78 + k:79 + k], rsg[0:64, :], ALU.mult, ALU.mult,
                                        [onh[h][1], r_rsg, r_pv], [r_mixO[o_i]])
                                    DMA("sp", mixS[64:128, c, :], mixO[0:64, o_i, :], reads=[r_mixO[o_i]], writes=[r_mix[c]])
                        pending.append(part2)
                    def cgroup(qc=qc, q0=q0):
                        for ct in range(2):
                            ACT(sqh[ct][0], ocT[:, ct, q0:q0 + 512], AF.Square, [r_ocT[ct][qc]], [sqh[ct][1]])
                        for ct in range(2):
                            MM(pb[5][:, :], ones, sqh[ct][0], ct == 0, ct == 1, [sqh[ct][1], r_cb], [r_pb[5]])
                        RSQ(rsg, pb[5][:, :], 1.0 / 256, [r_pb[5]], r_rsg)
                        for ct in range(2):
                            STT("dve", mixS[:, 6 + ct, :], ocT[:, ct, q0:q0 + 512], pcol(90 + ct), rsg, ALU.mult, ALU.mult,
                                [r_ocT[ct][qc], r_rsg, r_pv], [r_mix[6 + ct]])

                    def wout(qc=qc, l=l):
                        for ch in range(2):
                            for k in range(8):
                                DMA("pool", wo[:, k, :], w_out_d[l, rows[k]:rows[k] + 128, ch * 512:(ch + 1) * 512], writes=[r_wo[k]])
                            for t4 in range(4):
                                t = qc * 4 + t4
                                W_, rW = pb[5 + (t4 % 2)], r_pb[5 + (t4 % 2)]
                                for k in range(8):
                                    MM(W_[:, :], mixS[:, k, t4 * 128:(t4 + 1) * 128], wo[:, k, :], k == 0, k == 7, [r_mix[k], r_wo[k]], [rW])
                                xs = X[:, t, ch * 512:(ch + 1) * 512]
                                TT("dve", xs, xs, W_[:, :], ALU.add, [rW, rX[t]], [rX[t]])
                    pending.append(cgroup)
                    pending.append(wout)
                flush_pending()

                if dbg == "mix":
                    for t in range(NT):
                        dbg_ops.append(DMA("sp", dbg_d[:, t * 1024:(t + 1) * 1024], X[:, t, :], reads=[rX[t]]))

                P.barrier()
                AB.reset()
                AFa.reset()
                hT_ap, _ = AB.alloc(8 * S, "hTf")
                hT = hT_ap.rearrange("p (c t) -> p c t", c=8)
                r_hT = [Reg("hTf%d" % j) for j in range(4)]
                tmp = dict(gain=AFa.alloc(D, "gain"), junk=AB.alloc(D, "junk"), ht=[AB.alloc(D, "ht") for _ in range(2)])
                rms_to_hT([X[:, t, :] for t in range(NT)], rX, nffn_d[l:l + 1, :], tmp, hT, lambda t: r_hT[t // 4], lambda t: t * 128)
                gT_ap, _ = AB.alloc(8 * S, "gT")
                gT = gT_ap.rearrange("p (c t) -> p c t", c=8)
                r_gT = [[Reg("gT") for j in range(4)] for jj in range(8)]
                wd_ap, r_wd = AB.alloc(8 * D, "wd")
                wd = wd_ap.rearrange("p (c n) -> p c n", c=8)
                wu = []
                for i in range(2):
                    a, r = AB.alloc(8 * 256, "wu")
                    wu.append((a.rearrange("p (c k n) -> p c k n", c=8, k=2), r))
                ub = [[AFa.alloc(514, "ub") for _ in range(3)] for _ in range(2)]
                yb = [[AB.alloc_f32(512, "yb") for _ in range(3)] for _ in range(2)]
                sgl2 = [AB.alloc_f32(512, "sgl") for _ in range(2)]
                wu_l = w_up_d[l].rearrange("(c p) n -> p c n", p=128)
                f_groups = [(0, 8), (8, 8), (16, 6)]
                fi = 0
                ui = 0

                def load_wu(n):
                    if n >= NFT:
                        return
                    wU_, r_wU_ = wu[n % 2]
                    DMA("pool", wU_[:, :, 0, :], wu_l[:, :, n * 128:(n + 1) * 128], writes=[r_wU_])
                    DMA("pool", wU_[:, :, 1, :], wu_l[:, :, DFF + n * 128:DFF + (n + 1) * 128], writes=[r_wU_])
                load_wu(0)
                for (f0, nf) in f_groups:
                    DMA("pool", wd[:, 0:nf, :], w_dn_d[l, f0 * 128:(f0 + nf) * 128, :].rearrange("(c p) n -> p c n", p=128), writes=[r_wd])
                    units = []
                    for jj in range(nf):
                        wU, r_wU = wu[fi % 2]
                        fi += 1
                        for j in range(4):
                            units.append(dict(jj=jj, ft=f0 + jj, j=j, ui=ui, wU=wU, r_wU=r_wU))
                            ui += 1

                    def stA(u):
                        jj, ft, j, k, wU, r_wU = u["jj"], u["ft"], u["j"], u["ui"], u["wU"], u["r_wU"]
                        if j == 1:
                            load_wu(ft + 1)
                        g0 = j * 512
                        for kind in range(2):
                            if k % 3 == 2:
                                A_, rA = (pb[6][:, :], r_pb[6]) if kind == 0 else (ptr[:, :].bitcast(F32), r_ptr)
                            else:
                                A_, rA = pb[kind * 2 + (k % 3)][:, :], r_pb[kind * 2 + (k % 3)]
                            for c in range(8):
                                MM(A_, wU[:, c, kind, :], hT[:, c, g0:g0 + 512], c == 0, c == 7, [r_wU, r_hT[j]], [rA])
                            u_, r_u = ub[kind][k % 3]
                            up_, r_up = ub[kind][(k + 2) % 3]
                            y_, r_y = yb[kind][k % 3]
                            ftk = ft + 22 * kind
                            if j == 0:
                                MEMSET("pool", u_[:, 0:2], 0.0, [r_u])
                            else:
                                CP("pool", u_[:, 0:2], up_[:, 512:514], [r_up], [r_u])
                            ACOPY(u_[:, 2:514], A_, [rA], [r_u])
                            ACT(y_, A_, AF.Identity, [rA, r_pv], [r_y], bias=pcol(224 + ftk), scale=pcol(92 + 88 + ftk))

                    def stB(u):
                        ft, k = u["ft"], u["ui"]
                        for kind in range(2):
                            u_, r_u = ub[kind][k % 3]
                            y_, r_y = yb[kind][k % 3]
                            ftk = ft + 22 * kind
                            STT("dve", y_, u_[:, 1:513], pcol(92 + 44 + ftk), y_, ALU.mult, ALU.add, [r_u, r_y, r_pv], [r_y])
                            STT("dve", y_, u_[:, 0:512], pcol(92 + ftk), y_, ALU.mult, ALU.add, [r_u, r_y, r_pv], [r_y])

                    def stC(u):
                        jj, j, k = u["jj"], u["j"], u["ui"]
                        g0 = j * 512
                        sg, r_sg = sgl2[k % 2]
                        yg, r_yg = yb[0][k % 3]
                        yv, r_yv = yb[1][k % 3]
                        ACT(sg, yg, AF.Silu, [r_yg], [r_sg])
                        TT("dve", gT[:, jj, g0:g0 + 512], sg, yv, ALU.mult, [r_sg, r_yv], [r_gT[jj][j]])

                    n_u = len(units)
                    for i in range(n_u + 2):
                        if i < n_u:
                            stA(units[i])
                        if 0 <= i - 1 < n_u:
                            stB(units[i - 1])
                        if 0 <= i - 2 < n_u:
                            stC(units[i - 2])
                    for t in range(NT):
                        for ch in range(2):
                            W_, rW = pb[4 + ((t * 2 + ch) % 2)], r_pb[4 + ((t * 2 + ch) % 2)]
                            for jj in range(nf):
                                MM(W_[:, :], gT[:, jj, t * 128:(t + 1) * 128], wd[:, jj, ch * 512:(ch + 1) * 512], jj == 0, jj == nf - 1,
                                   [r_gT[jj][t // 4], r_wd], [rW])
                            xs = X[:, t, ch * 512:(ch + 1) * 512]
                            TT("dve", xs, xs, W_[:, :], ALU.add, [rW, rX[t]], [rX[t]])
            for t in range(NT):
                out_ops.append(DMA("sp", out_d[s, t * 128:(t + 1) * 128, :], X[:, t, :], reads=[rX[t]]))
        P.emit(nc, final_wait_ops=out_ops + dbg_ops)
    return nc


_CACHE = {}


def _prep(inputs):
    inp = {k: np.asarray(v) for k, v in inputs.items()}
    cst, gmask = _consts()
    pvec = np.stack([_pvec(inp, l) for l in range(DEPTH)], 0)
    shared = dict(
        w_in=np.ascontiguousarray(inp["w_in"], np.float32), w_mem_kv=np.ascontiguousarray(inp["w_mem_kv"], np.float32),
        w_conv_out=np.ascontiguousarray(inp["w_conv_out"], np.float32), w_out=np.ascontiguousarray(inp["w_out"], np.float32),
        w_up=np.ascontiguousarray(inp["w_up"], np.float32), w_down=np.ascontiguousarray(inp["w_down"], np.float32),
        norm_mix=np.ascontiguousarray(inp["norm_mix"], np.float32), norm_ffn=np.ascontiguousarray(inp["norm_ffn"], np.float32),
        mem_norm=np.ascontiguousarray(inp["mem_norm"], np.float32), pvec=pvec, cst=cst, gmask=gmask)
    in_maps = []
    for c in range(N_CORES):
        m = dict(shared)
        m["x"] = np.ascontiguousarray(inp["x"][2 * c:2 * c + 2], np.float32)
        m["mem"] = np.ascontiguousarray(inp["mem"][2 * c:2 * c + 2], np.float32)
        in_maps.append(m)
    return in_maps


def kernel(**inputs):
    in_maps = _prep(inputs)
    if "nc" not in _CACHE:
        _CACHE["nc"] = build()
    res = run_bass_kernel_spmd(_CACHE["nc"], in_maps, core_ids=list(range(N_CORES)))
    return np.concatenate([r["out"] for r in res.results], axis=0).astype(np.float32)
```
